# Optimizing a Trainium2 kernel written in Bass

```python
import jax, jax.numpy as jnp
from jax import lax
import numpy as np

D_MODEL = 1024
BATCH = 8
SEQ = 2048
DEPTH = 1
DEC_BATCH = 128
DEC_SEQ = 4
PAST_LEN = 16384
PAGE_SIZE = 128

N_META = 16
D_MIX = D_MODEL
NORM_EPS = 1e-6
GLA_WIDTH = D_MIX // 2
GLA_HEADS = 4
GLA_QK = GLA_WIDTH // 2
GLA_DK = GLA_QK // GLA_HEADS
GLA_DV = GLA_WIDTH // GLA_HEADS
GLA_GATE_RANK = 16
GLA_GATE_NORM = 16.0
GLA_CHUNK = 64
GLA_COLS = 2 * GLA_QK + 2 * GLA_WIDTH + GLA_GATE_RANK
RWKV_WIDTH = D_MIX - GLA_WIDTH
RWKV_HEAD = 64
RWKV_HEADS = RWKV_WIDTH // RWKV_HEAD
RWKV_DECAY_RANK = 64
RWKV_AAA_RANK = 64
RWKV_GATE_RANK = 128
RWKV_DECAY_SCALE = 0.606531
RWKV_GN_EPS = 64e-5
RWKV_COLS = 3 * RWKV_WIDTH + RWKV_DECAY_RANK + RWKV_AAA_RANK + RWKV_GATE_RANK
IN_COLS = GLA_COLS + RWKV_COLS
N_GROUPS = 4
EXPERTS_PER_GROUP = 8
N_EXPERTS = N_GROUPS * EXPERTS_PER_GROUP
TOP_K_INNER = 2
D_EXPERT = 512

kernel_name = 'hymba_gla_rwkv7_hmoe_step'

F32 = jnp.float32


def rmsnorm(x, g):
    xf = x.astype(F32)
    y = xf * lax.rsqrt(jnp.mean(xf * xf, axis=-1, keepdims=True) + NORM_EPS)
    return (y * g.astype(F32)).astype(x.dtype)


def gla_chunk(S, q, k, v, lg):
    L = q.shape[2]
    b = jnp.cumsum(lg, axis=2)
    causal = jnp.tril(jnp.ones((L, L), dtype=bool))[:, :, None]
    diff = b[:, :, :, None, :] - b[:, :, None, :, :]
    decay = jnp.where(causal, jnp.exp(jnp.where(causal, diff, 0.0)), 0.0)
    scores = jnp.einsum('bhid,bhjd,bhijd->bhij', q, k, decay)
    o = jnp.einsum('bhij,bhje->bhie', scores, v) + jnp.einsum('bhid,bhde->bhie', q * jnp.exp(b), S)
    b_last = b[:, :, -1:, :]
    S_new = jnp.exp(b_last[:, :, 0, :])[..., None] * S + jnp.einsum('bhjd,bhje->bhde', k * jnp.exp(b_last - b), v)
    return o, S_new


def gla_scan(S0, q, k, v, lg, n_lead):
    B, H, T, _ = q.shape
    outs = []
    S = S0
    if n_lead > 0:
        o, S = gla_chunk(S, q[:, :, :n_lead], k[:, :, :n_lead], v[:, :, :n_lead], lg[:, :, :n_lead])
        outs.append(o)
    rest = T - n_lead
    C = GLA_CHUNK if rest % GLA_CHUNK == 0 else rest
    nc = rest // C

    def to_chunks(a):
        return a[:, :, n_lead:].reshape(B, H, nc, C, a.shape[-1]).transpose(2, 0, 1, 3, 4)

    def step(S_c, xs):
        o_c, S_c = gla_chunk(S_c, *xs)
        return S_c, o_c

    S, oc = lax.scan(step, S, (to_chunks(q), to_chunks(k), to_chunks(v), to_chunks(lg)))
    outs.append(oc.transpose(1, 2, 0, 3, 4).reshape(B, H, rest, v.shape[-1]))
    return jnp.concatenate(outs, axis=2), S


def rwkv_scan(S0, r, w, k, v, kk, a):
    def step(S, xs):
        r_t, w_t, k_t, v_t, kk_t, a_t = xs
        sab = jnp.einsum('bhij,bhj->bhi', S, kk_t)
        S = S * w_t[:, :, None, :] - sab[..., None] * (kk_t * a_t)[:, :, None, :] + v_t[..., None] * k_t[:, :, None, :]
        y = jnp.einsum('bhij,bhj->bhi', S, r_t)
        return S, y
    xs = (jnp.moveaxis(r, 1, 0), jnp.moveaxis(w, 1, 0), jnp.moveaxis(k, 1, 0),
          jnp.moveaxis(v, 1, 0), jnp.moveaxis(kk, 1, 0), jnp.moveaxis(a, 1, 0))
    S, ys = lax.scan(step, S0, xs)
    return jnp.moveaxis(ys, 0, 1), S


def mixer(n, S_gla, S_rwkv, shift, n_lead, w_in, gla_gate_w2, gla_gate_b, gla_norm,
          mu, w0, w2, a0, a2, g2, k_k, k_a, r_k, ln_w, ln_b, w_out):
    B, T, _ = n.shape
    p = n @ w_in
    pg, pr = p[..., :GLA_COLS], p[..., GLA_COLS:]
    gq, gk, gv, gg, gl = jnp.split(pg, [GLA_QK, 2 * GLA_QK, 2 * GLA_QK + GLA_WIDTH, 2 * GLA_QK + 2 * GLA_WIDTH], axis=-1)
    lg = jax.nn.log_sigmoid((gl @ gla_gate_w2 + gla_gate_b).astype(F32)) / GLA_GATE_NORM

    def heads(t, d):
        return t.reshape(B, T, GLA_HEADS, d).transpose(0, 2, 1, 3).astype(F32)

    o, S_gla_new = gla_scan(S_gla.astype(F32), heads(gq, GLA_DK) * (GLA_DK ** -0.5), heads(gk, GLA_DK),
                            heads(gv, GLA_DV), heads(lg, GLA_DK), n_lead)
    o = o.transpose(0, 2, 1, 3)
    o = o * lax.rsqrt(jnp.mean(o * o, axis=-1, keepdims=True) + NORM_EPS) * gla_norm.astype(F32)
    o_gla = o.reshape(B, T, GLA_WIDTH) * jax.nn.silu(gg.astype(F32))
    prev = jnp.concatenate([shift[:, None, :].astype(pr.dtype), pr[:, :-1]], axis=1)
    xm = pr + (prev - pr) * mu
    rr, rk, rv, wl, al, gl2 = jnp.split(xm, [RWKV_WIDTH, 2 * RWKV_WIDTH, 3 * RWKV_WIDTH,
                                            3 * RWKV_WIDTH + RWKV_DECAY_RANK,
                                            3 * RWKV_WIDTH + RWKV_DECAY_RANK + RWKV_AAA_RANK], axis=-1)
    logw = -RWKV_DECAY_SCALE * jax.nn.sigmoid((w0 + jnp.tanh(wl) @ w2).astype(F32))
    aa = jax.nn.sigmoid((a0 + al @ a2).astype(F32))
    gate = (jax.nn.sigmoid(gl2) @ g2).astype(F32)

    def rh(t):
        return t.reshape(B, T, RWKV_HEADS, RWKV_HEAD).astype(F32)

    r_h, k_h, v_h, a_h, w_h = rh(rr), rh(rk), rh(rv), rh(aa), jnp.exp(rh(logw))
    kk = k_h * k_k.astype(F32).reshape(RWKV_HEADS, RWKV_HEAD)
    kk = kk / jnp.maximum(jnp.sqrt(jnp.sum(kk * kk, axis=-1, keepdims=True)), 1e-12)
    k_h = k_h * (1.0 + (a_h - 1.0) * k_a.astype(F32).reshape(RWKV_HEADS, RWKV_HEAD))
    y, S_rwkv_new = rwkv_scan(S_rwkv.astype(F32), r_h, w_h, k_h, v_h, kk, a_h)
    mean = jnp.mean(y, axis=-1, keepdims=True)
    var = jnp.mean(jnp.square(y - mean), axis=-1, keepdims=True)
    y = (y - mean) * lax.rsqrt(var + RWKV_GN_EPS) * ln_w.astype(F32).reshape(RWKV_HEADS, RWKV_HEAD) \
        + ln_b.astype(F32).reshape(RWKV_HEADS, RWKV_HEAD)
    y = y + jnp.sum(r_h * k_h * r_k.astype(F32), axis=-1, keepdims=True) * v_h
    o_rwkv = y.reshape(B, T, RWKV_WIDTH) * gate
    out = jnp.concatenate([o_gla, o_rwkv], axis=-1).astype(n.dtype) @ w_out
    return out, S_gla_new, S_rwkv_new, pr[:, -1]


def hier_moe(x, rg_w, rg_b, re_w, re_b, w1, w3, w2):
    pg = jax.nn.softmax((x @ rg_w + rg_b).astype(F32), axis=-1)
    p_top, g_idx = lax.top_k(pg, 1)
    el = (jnp.einsum('td,gde->tge', x, re_w) + re_b).astype(F32)
    el_sel = jnp.take_along_axis(el, g_idx[:, :, None], axis=1)[:, 0]
    pe = jax.nn.softmax(el_sel, axis=-1)
    w_top, e_idx = lax.top_k(pe, TOP_K_INNER)
    w_top = w_top / jnp.sum(w_top, axis=-1, keepdims=True) * p_top
    eidx = g_idx * EXPERTS_PER_GROUP + e_idx
    combine = jnp.sum(jax.nn.one_hot(eidx, N_EXPERTS, dtype=F32) * w_top[..., None], axis=1)
    y = jnp.zeros(x.shape, F32)
    for e in range(N_EXPERTS):
        h = jax.nn.silu(x @ w1[e]) * (x @ w3[e])
        y = y + combine[:, e:e + 1] * (h @ w2[e]).astype(F32)
    return y.astype(x.dtype)


def setup_inputs(seed: int = 0) -> dict:
    key = jax.random.key(seed)
    ks = jax.random.split(key, 32)
    L = DEPTH

    def nrm(i, shape, s):
        return s * jax.random.normal(ks[i], shape, F32)

    def gain(i, shape):
        return 1.0 + 0.02 * jax.random.normal(ks[i], shape, F32)

    return {
        'x_prompt': nrm(0, (BATCH, SEQ, D_MODEL), 1.0),
        'x_sample': nrm(1, (DEC_BATCH, DEC_SEQ, D_MODEL), 1.0),
        'state_gla': nrm(2, (L, DEC_BATCH, GLA_HEADS, GLA_DK, GLA_DV), 0.5),
        'state_rwkv': nrm(3, (L, DEC_BATCH, RWKV_HEADS, RWKV_HEAD, RWKV_HEAD), 0.5),
        'state_shift': nrm(4, (L, DEC_BATCH, RWKV_COLS), 1.0),
        'meta_tokens': nrm(5, (N_META, D_MODEL), 1.0),
        'norm_mix': gain(6, (L, D_MODEL)),
        'w_in': nrm(7, (L, D_MODEL, IN_COLS), D_MODEL ** -0.5),
        'gla_gate_w2': nrm(8, (L, GLA_GATE_RANK, GLA_QK), GLA_GATE_RANK ** -0.5),
        'gla_gate_b': nrm(9, (L, GLA_QK), 0.5),
        'gla_norm': gain(10, (L, GLA_DV)),
        'rwkv_mu': jax.random.uniform(ks[11], (L, RWKV_COLS), F32),
        'rwkv_w0': nrm(12, (L, RWKV_WIDTH), 0.5),
        'rwkv_w2': nrm(13, (L, RWKV_DECAY_RANK, RWKV_WIDTH), RWKV_DECAY_RANK ** -0.5),
        'rwkv_a0': nrm(14, (L, RWKV_WIDTH), 0.1),
        'rwkv_a2': nrm(15, (L, RWKV_AAA_RANK, RWKV_WIDTH), RWKV_AAA_RANK ** -0.5),
        'rwkv_g2': nrm(16, (L, RWKV_GATE_RANK, RWKV_WIDTH), RWKV_GATE_RANK ** -0.5),
        'rwkv_kk': 0.85 + nrm(17, (L, RWKV_WIDTH), 0.1),
        'rwkv_ka': 1.0 + nrm(18, (L, RWKV_WIDTH), 0.1),
        'rwkv_rk': nrm(19, (L, RWKV_HEADS, RWKV_HEAD), 0.1),
        'rwkv_ln_w': gain(20, (L, RWKV_WIDTH)),
        'rwkv_ln_b': nrm(21, (L, RWKV_WIDTH), 0.01),
        'w_out': nrm(22, (L, D_MIX, D_MODEL), D_MIX ** -0.5),
        'norm_ffn': gain(23, (L, D_MODEL)),
        'router_group_w': nrm(24, (L, D_MODEL, N_GROUPS), D_MODEL ** -0.5),
        'router_group_b': nrm(25, (L, N_GROUPS), 0.01),
        'router_expert_w': nrm(26, (L, N_GROUPS, D_MODEL, EXPERTS_PER_GROUP), D_MODEL ** -0.5),
        'router_expert_b': nrm(27, (L, N_GROUPS, EXPERTS_PER_GROUP), 0.01),
        'moe_w1': nrm(28, (L, N_EXPERTS, D_MODEL, D_EXPERT), D_MODEL ** -0.5),
        'moe_w3': nrm(29, (L, N_EXPERTS, D_MODEL, D_EXPERT), D_MODEL ** -0.5),
        'moe_w2': nrm(30, (L, N_EXPERTS, D_EXPERT, D_MODEL), D_EXPERT ** -0.5),
        'norm_final': gain(31, (D_MODEL,)),
    }


def reference(x_prompt, x_sample, state_gla, state_rwkv, state_shift, meta_tokens, norm_mix, w_in,
              gla_gate_w2, gla_gate_b, gla_norm, rwkv_mu, rwkv_w0, rwkv_w2, rwkv_a0, rwkv_a2, rwkv_g2,
              rwkv_kk, rwkv_ka, rwkv_rk, rwkv_ln_w, rwkv_ln_b, w_out, norm_ffn, router_group_w,
              router_group_b, router_expert_w, router_expert_b, moe_w1, moe_w3, moe_w2, norm_final):
    b_p = x_prompt.shape[0]
    meta = jnp.broadcast_to(meta_tokens.astype(x_prompt.dtype)[None], (b_p, N_META, D_MODEL))
    hp = jnp.concatenate([meta, x_prompt], axis=1)
    hs = x_sample
    gla_p, rwkv_p, shift_p, gla_s, rwkv_s, shift_s = [], [], [], [], [], []
    for l in range(DEPTH):
        mix_w = (w_in[l], gla_gate_w2[l], gla_gate_b[l], gla_norm[l], rwkv_mu[l], rwkv_w0[l], rwkv_w2[l],
                 rwkv_a0[l], rwkv_a2[l], rwkv_g2[l], rwkv_kk[l], rwkv_ka[l], rwkv_rk[l], rwkv_ln_w[l],
                 rwkv_ln_b[l], w_out[l])
        z_gla = jnp.zeros((b_p, GLA_HEADS, GLA_DK, GLA_DV), F32)
        z_rwkv = jnp.zeros((b_p, RWKV_HEADS, RWKV_HEAD, RWKV_HEAD), F32)
        z_shift = jnp.zeros((b_p, RWKV_COLS), hp.dtype)
        mp, sg_p, sr_p, sh_p = mixer(rmsnorm(hp, norm_mix[l]), z_gla, z_rwkv, z_shift, N_META, *mix_w)
        ms, sg_s, sr_s, sh_s = mixer(rmsnorm(hs, norm_mix[l]), state_gla[l], state_rwkv[l], state_shift[l], 0, *mix_w)
        hp = hp + mp
        hs = hs + ms
        n_tok_p = hp.shape[0] * hp.shape[1]
        tok = jnp.concatenate([hp.reshape(n_tok_p, D_MODEL), hs.reshape(-1, D_MODEL)], axis=0)
        f = hier_moe(rmsnorm(tok, norm_ffn[l]), router_group_w[l], router_group_b[l], router_expert_w[l],
                     router_expert_b[l], moe_w1[l], moe_w3[l], moe_w2[l])
        hp = hp + f[:n_tok_p].reshape(hp.shape)
        hs = hs + f[n_tok_p:].reshape(hs.shape)
        gla_p.append(sg_p.astype(state_gla.dtype))
        rwkv_p.append(sr_p.astype(state_rwkv.dtype))
        shift_p.append(sh_p.astype(state_shift.dtype))
        gla_s.append(sg_s.astype(state_gla.dtype))
        rwkv_s.append(sr_s.astype(state_rwkv.dtype))
        shift_s.append(sh_s.astype(state_shift.dtype))
    y_prompt = rmsnorm(hp[:, N_META:], norm_final)
    y_sample = rmsnorm(hs, norm_final)
    return (y_prompt, y_sample, jnp.stack(gla_p), jnp.stack(rwkv_p), jnp.stack(shift_p),
            jnp.stack(gla_s), jnp.stack(rwkv_s), jnp.stack(shift_s))
```

```python
import os
import numpy as np
from contextlib import ExitStack
import concourse.bass as bass
import concourse.mybir as mybir
from concourse.bass_utils import run_bass_kernel_spmd

F32 = mybir.dt.float32
BF16 = mybir.dt.bfloat16
ALU = mybir.AluOpType
AF = mybir.ActivationFunctionType
AX = mybir.AxisListType

NDMA = 24
NCORES = 8
D = 1024
GLA_COLS = 1552
RW_COLS = 1792
IN_COLS = 3344
NE = 32
DE = 512
NPT = 16
NSLOT = 17
NTOK = 2048 + 64
DECAY = -0.606531
MOE_LIMIT = int(os.environ.get("K_MOE_LIMIT", "32"))
K_NT = int(os.environ.get("K_NT", "18"))
K_STOP = int(os.environ.get("K_STOP", "99"))


class _Stop(Exception):
    pass


def _chk(n):
    if K_STOP <= n:
        raise _Stop()


K_PH = int(os.environ.get("K_PH", "9"))


class Sched:
    def __init__(self, nc, es):
        self.nc = nc
        self.engs = ['pe', 'act', 'dve', 'pool', 'sp']
        self.ops = {e: [] for e in self.engs}
        self.count = {}
        self.seen = {e: {} for e in self.engs}
        self.last_w = {}
        self.readers = {}
        self.sems = {}
        for k in ['pe', 'act', 'dve', 'pool']:
            self.sems[k] = es.enter_context(nc.semaphore('s_' + k))
            self.count[k] = 0
        for i in range(NDMA):
            k = ('dma', i)
            self.sems[k] = es.enter_context(nc.semaphore('s_dma%d' % i))
            self.count[k] = 0
        self.dma_rr = 0

    keymap = {}

    @staticmethod
    def key(x):
        if isinstance(x, (str, tuple)):
            return x
        n = x.tensor.name if hasattr(x, 'tensor') else x.name
        return Sched.keymap.get(n, n)

    def op(self, eng, fn, reads, writes, dma=False):
        reads = [self.key(r) for r in reads]
        writes = [self.key(w) for w in writes]
        deps = []
        for b in reads:
            if b in self.last_w:
                deps.append(self.last_w[b])
            if isinstance(b, str) and b.startswith('ps'):
                deps.extend(ev for ev in self.readers.get(b, []) if ev[0] != eng)
        for b in writes:
            if b in self.last_w:
                deps.append(self.last_w[b])
            deps.extend(self.readers.get(b, []))
        seen = self.seen[eng]
        wd = {}
        for (k, v) in deps:
            if k == 'pe' and eng == 'pe':
                continue
            if seen.get(k, 0) >= v:
                continue
            seen[k] = v
            wd[k] = max(wd.get(k, 0), v)
        if dma:
            k = ('dma', self.dma_rr % NDMA)
            self.dma_rr += 1
            if self.count[k] > 0 and seen.get(k, 0) < self.count[k]:
                seen[k] = self.count[k]
                wd[k] = self.count[k]
            self.count[k] += 16
            ev = (k, self.count[k])
            inc = (k, 16)
        else:
            self.count[eng] += 1
            ev = (eng, self.count[eng])
            inc = (eng, 1)
        self.ops[eng].append((fn, list(wd.items()), inc))
        for b in writes:
            self.last_w[b] = ev
            self.readers[b] = []
        for b in reads:
            if b not in writes:
                self.readers.setdefault(b, []).append(ev)
        return ev

    def barrier(self):
        snap = dict(self.count)
        for e in self.engs:
            waits = []
            for k, v in snap.items():
                if v == 0 or k == e:
                    continue
                if self.seen[e].get(k, 0) >= v:
                    continue
                self.seen[e][k] = v
                waits.append((k, v))
            if waits:
                self.ops[e].append((None, waits, None))

    def finish(self):
        waits = [(k, v) for k, v in self.count.items() if v > 0]
        self.ops['sp'].append((None, waits, None))

    def emit(self):
        nc = self.nc
        engobj = {'pe': 'tensor', 'act': 'scalar', 'dve': 'vector', 'pool': 'gpsimd', 'sp': 'sync'}
        sems = self.sems
        with nc.allow_low_precision("bf16 matmul operands by design"), nc.Block() as block:
            for e in self.engs:
                def body(eng, ops=self.ops[e]):
                    for fn, waits, inc in ops:
                        for k, v in waits:
                            eng.wait_ge(sems[k], v)
                        if fn is None:
                            continue
                        ins = fn(eng)
                        ins.then_inc(sems[inc[0]], inc[1])
                getattr(block, engobj[e])(body)
        self.ops = {e: [] for e in self.engs}

    def mm(self, out, lhsT, rhs, start=True, stop=True):
        r = [lhsT, rhs] + ([] if start else [out])
        return self.op('pe', lambda e: e.matmul(out, lhsT, rhs, start=start, stop=stop), r, [out])

    def tr(self, out, in_, ident):
        return self.op('pe', lambda e: e.transpose(out, in_, ident), [in_, ident], [out])

    def act(self, out, in_, func=None, bias=0.0, scale=1.0, accum=None):
        func = AF.Copy if func is None else func
        r = [in_] + [x for x in (bias, scale) if not isinstance(x, (int, float))]
        w = [out] + ([accum] if accum is not None else [])
        kw = {}
        if accum is not None:
            kw['accum_out'] = accum
        return self.op('act', lambda e: e.activation(out, in_, func, bias=bias, scale=scale, **kw), r, w)

    def tt(self, eng, out, a, b, op):
        return self.op(eng, lambda e: e.tensor_tensor(out, a, b, op), [a, b], [out])

    def ts(self, eng, out, a, s1, s2=None, op0=ALU.mult, op1=None):
        r = [a] + [x for x in (s1, s2) if x is not None and not isinstance(x, (int, float))]
        if op1 is None:
            return self.op(eng, lambda e: e.tensor_scalar(out, a, s1, s2, op0), r, [out])
        return self.op(eng, lambda e: e.tensor_scalar(out, a, s1, s2, op0, op1), r, [out])

    def stt(self, eng, out, a, s, b, op0, op1):
        r = [a, b] + ([] if isinstance(s, (int, float)) else [s])
        eng = 'dve'
        return self.op(eng, lambda e: e.scalar_tensor_tensor(out, a, s, b, op0, op1), r, [out])

    def cp(self, eng, out, a):
        if eng == 'act':
            return self.act(out, a)
        return self.op(eng, lambda e: e.tensor_copy(out, a), [a], [out])

    def recip(self, out, a):
        return self.op('dve', lambda e: e.reciprocal(out, a), [a], [out])

    def memset(self, eng, out, val):
        return self.op(eng, lambda e: e.memset(out, val), [], [out])

    def dma(self, out, in_, eng='sp', **kw):
        return self.op(eng, lambda e: e.dma_start(out=out, in_=in_, **kw), [in_], [out], dma=True)


def bc(ap, shape, axis):
    return ap.unsqueeze(axis).to_broadcast(list(shape))


def build():
    nc = bass.Bass("TRN2", target_bir_lowering=False)
    din = lambda n, s: nc.dram_tensor(n, list(s), F32, kind="ExternalInput").ap()
    dout = lambda n, s: nc.dram_tensor(n, list(s), F32, kind="ExternalOutput").ap()
    x_d = din("x", [2048, D]); xs_d = din("xs", [64, D]); meta_d = din("meta", [16, D])
    sg_d = din("sg", [16, 4, 64, 128]); sr_d = din("sr", [16, 8, 64, 64]); ssh_d = din("ssh", [16, RW_COLS])
    win_d = din("win", [128, 8, IN_COLS])
    wout_d = din("wout", [128, 8, D])
    gw2_d = din("gw2", [16, 256]); gb_d = din("gb", [1, 256])
    w2_d = din("w2", [64, 512]); a2_d = din("a2", [64, 512]); g2_d = din("g2", [128, 512])
    w0r_d = din("w0r", [1, 512])
    v128_d = din("v128", [128, 43]); v64_d = din("v64", [64, 82])
    wr_d = din("wr", [128, 8, 36]); rb_d = din("rb", [1, 36])
    nf_d = din("nf", [1, D])
    w1_d = din("w1", [NE, 128, 8, DE]); w3_d = din("w3", [NE, 128, 8, DE]); w2m_d = din("w2m", [NE, 128, 4, D])
    cst_d = din("cst", [128, 10, 128])
    yp_d = dout("yp", [2048, D]); ys_d = dout("ys", [64, D])
    glap_d = dout("glap", [4, 64, 128]); rwp_d = dout("rwp", [8, 64, 64]); shp_d = dout("shp", [1, RW_COLS])
    glas_d = dout("glas", [16, 4, 64, 128]); rws_d = dout("rws", [16, 8, 64, 64]); shs_d = dout("shs", [16, RW_COLS])

    with ExitStack() as es:
        S = Sched(nc, es)
        sbG = lambda n, s, d=F32: es.enter_context(nc.sbuf_tensor("s_" + n, list(s), d))
        cst = sbG("cst", [128, 10, 128])
        identf = cst[:, 0, :]; triu_i = cst[:, 1, :]; triu_s = cst[:, 2, :]; tril_s = cst[:, 3, :]; onesf = cst[:, 4, :]
        blk_i = cst[:, 5, :]; blk_us = cst[:, 6, :]; blk_ls = cst[:, 7, :]; ones64 = cst[:, 8, :]
        identb = sbG("identb", [128, 128], BF16)
        actG = sbG("actG", [128, 4, NTOK], BF16)
        actR = sbG("actR", [128, 4, NTOK], BF16)
        v128 = sbG("v128", [128, 43]); v64 = sbG("v64", [64, 82])
        S.dma(cst[:], cst_d); S.dma(v128[:], v128_d); S.dma(v64[:], v64_d)
        S.cp('dve', identb[:], identf)
        g_mix = v128[:, 0:8]; g_ffn = v128[:, 8:16]; gnorm = v128[:, 16:17]; mu_g = v128[:, 17:18]
        mu128 = v128[:, 18:27]; a0c128 = v128[:, 27:31]; kkw128 = v128[:, 31:35]; kac128 = v128[:, 35:39]; rkc128 = v128[:, 39:43]
        blkones = cst[:, 9, :]
        mu64 = v64[:, 0:26]; a0c = v64[:, 34:42]; kkw = v64[:, 42:50]; kac = v64[:, 50:58]
        rkc = v64[:, 58:66]; lnw = v64[:, 66:74]; lnb = v64[:, 74:82]

        winb_d = nc.dram_tensor("winb_scr", [128, 8, IN_COLS], BF16).ap()
        with ExitStack() as p1:
            sb = lambda n, s, d=F32: p1.enter_context(nc.sbuf_tensor("s_" + n, list(s), d))
            ps = lambda n, s, d=F32: p1.enter_context(nc.psum_tensor(n, list(s), d))
            psF = [ps("psF%d" % i, [128, 512]) for i in range(4)]
            psB = [ps("psB%d" % i, [128, 1024], BF16) for i in range(2)]
            psW = ps("psW", [128, 1024])
            rr = {'a': 0, 'b': 0}

            def pfA():
                rr['a'] += 1
                return psF[rr['a'] % 2]

            def pfB():
                rr['b'] += 1
                return psF[2 + rr['b'] % 2]

            sc = [sb("sc%d" % i, [128, 512]) for i in range(8)]
            s2 = [sb("s2_%d" % i, [128, 512]) for i in range(5)]
            v64v = lambda t: t[0:64, :].rearrange("p (h t) -> p h t", h=4)

            cb16 = [sb("cb16_%d" % i, [128, 512], BF16) for i in range(4)]
            pieces = [(k, c0, min(512, IN_COLS - c0)) for k in range(8) for c0 in range(0, IN_COLS, 512)]
            stin = sc[0:6]
            for i in range(min(6, len(pieces))):
                k, c0, w_ = pieces[i]
                S.dma(stin[i % 6][:, 0:w_], win_d[:, k, c0:c0 + w_])
            for i, (k, c0, w_) in enumerate(pieces):
                S.cp(['dve', 'pool'][i % 2], cb16[i % 4][:, 0:w_], stin[i % 6][:, 0:w_])
                S.op('act', lambda e, o=winb_d[:, k, c0:c0 + w_], a=cb16[i % 4][:, 0:w_]: e.dma_start(out=o, in_=a),
                     [cb16[i % 4]], [('winb_piece', i)], dma=True)
                if i + 6 < len(pieces):
                    k2, c2, w2_ = pieces[i + 6]
                    S.dma(stin[(i + 6) % 6][:, 0:w2_], win_d[:, k2, c2:c2 + w2_])
            S.barrier()
            wbuf = [sb("wbuf%d" % i, [128, 8, 256], BF16) for i in range(3)]
            wi = [0]
            gw2f = sb("gw2f", [16, 256]); gbf = sb("gbf", [1, 256])
            w2b = sb("w2b", [64, 512], BF16); a2b128 = sb("a2b128", [128, 512], BF16); g2b = sb("g2b", [128, 512], BF16)
            w0r = sb("w0r", [1, 512])
            S.dma(gw2f[:], gw2_d); S.dma(gbf[:], gb_d)
            S.dma(sc[4][0:64, :], w2_d); S.cp('pool', w2b[:], sc[4][0:64, :])
            S.dma(sc[5][64:128, :], a2_d); S.cp('pool', a2b128[64:128, :], sc[5][64:128, :])
            S.dma(sc[6][:, :], g2_d); S.cp('pool', g2b[:], sc[6][:, :])
            S.dma(w0r[:], w0r_d)
            omu64 = sb("omu64", [64, 26]); omu_g = sb("omu_g", [128, 1]); omu128 = sb("omu128", [128, 9])
            S.ts('dve', omu128[:], mu128, -1.0, 1.0, ALU.mult, ALU.add)
            v128v = lambda t: t[:, :].rearrange("p (h t) -> p h t", h=4)
            S.ts('dve', omu64[:], mu64, -1.0, 1.0, ALU.mult, ALU.add)
            S.ts('dve', omu_g[:], mu_g, -1.0, 1.0, ALU.mult, ALU.add)
            m4 = sb("m4", [128, 4, 128])
            for q in range(4):
                S.cp('pool', m4[:, q, :], triu_s if q % 2 == 0 else triu_i)
            xt = sb("xt", [128, D]); xsb = sb("xsb", [128, D], BF16)
            stat1 = sb("stat1", [128, 4])
            nT = sb("nT", [128, 8, 128], BF16)
            carry = [sb("carry%d" % i, [64, 26]) for i in range(2)]
            carryG = [sb("carryG%d" % i, [128, 1]) for i in range(2)]
            carry128 = [sb("carry128_%d" % i, [128, 9]) for i in range(2)]
            shT128 = sb("shT128", [128, 9, 16])
            shT = sb("shT", [64, 26, 16]); shTG = sb("shTG", [128, 16])
            xr = sb("xr", [128, 4, 128]); xk = sb("xk", [128, 4, 128])
            shrow = sb("shrow", [64, RW_COLS])
            xwa = sb("xwa", [128, 1, 128]); xmG = sb("xmG", [128, 128]); t4g = sb("t4g", [128, 128])
            glT = sb("glT", [16, 128])
            twl = sb("twl", [64, 128]); twlb = sb("twlb", [64, 128], BF16); alb128 = sb("alb128", [128, 128], BF16)
            blkonesb = sb("blkonesb", [128, 128], BF16)
            S.cp('pool', blkonesb[:], blkones)
            sgl = sb("sgl", [128, 128]); slw = sb("slw", [128, 512])
            HB = []
            for i in range(2):
                n_ = lambda s_: "%s_%d" % (s_, i)
                HB.append(dict(
                    vT=sb(n_("vT"), [128, 4, 128], BF16), ggT=sb(n_("ggT"), [128, 4, 128]),
                    qs=sb(n_("qs"), [64, 4, 128], BF16), ks=sb(n_("ks"), [64, 4, 128], BF16), khT=sb(n_("khT"), [64, 4, 128], BF16),
                    gam=sb(n_("gam"), [64, 4, 16]), WCs=sb(n_("WCs"), [64, 8, 16]), vTb=sb(n_("vTb"), [64, 8, 128], BF16),
                    ARt=sb(n_("ARt"), [128, 4, 2, 128], BF16), BtT=sb(n_("BtT"), [128, 4, 128], BF16), KtT=sb(n_("KtT"), [128, 4, 128], BF16),
                    rkr=sb(n_("rkr"), [128, 4, 128], BF16), sglb=sb(n_("sglb"), [128, 128], BF16)))
            Vg = sb("Vg", [128, 768], BF16); scT = sb("scT", [128, 4, 128], BF16)
            ogT = sb("ogT", [128, 4, 128]); Sg = sb("Sg", [64, 4, 128]); Sgb = sb("Sgb", [64, 4, 128], BF16)
            psB1f = psB[1][:, :].bitcast(F32)
            rhsPQ = sb("rhsPQ", [128, 8, 2, 64], BF16); tokB = sb("tokB", [128, 8, 64], BF16)
            tokKV = sb("tokKV", [128, 2, 8, 64], BF16)
            Mh = [sb("Mh%d" % h, [128, 4, 128], BF16) for h in range(8)]
            MhS = sb("MhS", [16, 8, 4, 16], BF16)
            YZ = [sb("YZ%d" % i, [128, 4, 2, 128], BF16) for i in range(2)]
            TT = [sb("TT%d" % i, [128, 4, 128], BF16) for i in range(2)]
            PQ = sb("PQ", [128, 8, 2, 64], BF16)
            GT = sb("GT", [64, 8, 64]); HW = sb("HW", [64, 8, 64]); ATb = sb("ATb", [64, 8, 128], BF16)
            Sr = sb("Sr", [64, 8, 64]); Srb = sb("Srb", [64, 8, 64], BF16)
            yT = sb("yT", [64, 8, 128]); oRo = sb("oRo", [64, 4, 128], BF16)
            onesb = sb("onesb", [64, 64], BF16)
            S.cp('pool', onesb[:], onesf[0:64, 0:64])

            for cb in carry:
                S.memset('pool', cb[:], 0.0)
            for cb in carryG:
                S.memset('pool', cb[:], 0.0)
            for cb in carry128:
                S.memset('pool', cb[:], 0.0)
            S.memset('dve', Sg[:], 0.0); S.memset('dve', Sr[:], 0.0)
            S.memset('pool', Sgb[:], 0.0); S.memset('pool', Srb[:], 0.0)
            S.dma(shrow[0:16, :], ssh_d)
            for g in range(26):
                S.tr(psW[0:64, g * 16:(g + 1) * 16], shrow[0:16, g * 64:(g + 1) * 64], identf[0:16, 0:16])
            S.cp('act', shT[:], psW[0:64, 0:416].rearrange("p (g s) -> p g s", g=26))
            p_ = pfA()
            for gi, c0 in enumerate([0, 128, 256, 384, 512, 640, 768, 896, 1536]):
                S.tr(p_[:, gi * 16:(gi + 1) * 16], shrow[0:16, c0:c0 + 128], identf[0:16, 0:16])
            S.cp('act', shT128[:], p_[:, 0:144].rearrange("p (g s) -> p g s", g=9))
            p_ = pfA()
            S.tr(p_[:, 0:16], shrow[0:16, 1664:1792], identf[0:16, 0:16])
            S.cp('act', shTG[:], p_[:, 0:16])
            tcount = [0]

            def tile_info(slot):
                sample = (slot == 16)
                Tn = 16 if slot < 0 else (64 if sample else 128)
                tok0 = 2048 if sample else slot * 128
                return sample, Tn, tok0

            def st1(slot, H):
                sample, Tn, tok0 = tile_info(slot)
                if slot < 0:
                    src = meta_d
                elif sample:
                    src = xs_d
                else:
                    src = x_d[slot * 128:(slot + 1) * 128, :]
                mi = blk_i if sample else triu_i
                mus = blk_us if sample else triu_s
                mls = blk_ls if sample else tril_s
                c_old = carry[tcount[0] % 2]; c_new = carry[(tcount[0] + 1) % 2]
                cg_old = carryG[tcount[0] % 2]; cg_new = carryG[(tcount[0] + 1) % 2]
                c128_old = carry128[tcount[0] % 2]; c128_new = carry128[(tcount[0] + 1) % 2]
                tcount[0] += 1
                vT = H['vT']; ggT = H['ggT']; qs = H['qs']; ks = H['ks']; khT = H['khT']; gam = H['gam']; WCs = H['WCs']
                vTb = H['vTb']; ARt = H['ARt']; BtT = H['BtT']; KtT = H['KtT']; rkr = H['rkr']; sglb = H['sglb']
                S.dma(xt[0:Tn, :], src)
                S.memset('pool', stat1[0:Tn, 0:1], 0.0)
                S.act(xsb[0:Tn, :], xt[0:Tn, :], AF.Square, accum=stat1[0:Tn, 0:1])
                S.act(stat1[0:Tn, 1:2], stat1[0:Tn, 0:1], AF.Ln, scale=1.0 / D, bias=1e-6)
                S.act(stat1[0:Tn, 2:3], stat1[0:Tn, 1:2], AF.Exp, scale=-0.5)
                S.ts('dve', xsb[0:Tn, :], xt[0:Tn, :], stat1[0:Tn, 2:3], None, ALU.mult)
                p_ = psB[0]
                for k in range(8):
                    S.tr(p_[:, k * 128:k * 128 + Tn], xsb[0:Tn, k * 128:(k + 1) * 128], identb[0:Tn, 0:Tn])
                S.tt('dve', nT[:, :, 0:Tn], p_[:, :].rearrange("p (k t) -> p k t", k=8)[:, :, 0:Tn],
                     bc(g_mix, [128, 8, Tn], 2), ALU.mult)
                yield

                def proj(c0, M, ngroups, dst_fn):
                    per = 256 // M if M >= 64 else 1
                    g = 0
                    while g < ngroups:
                        n = min(per, ngroups - g)
                        wb = wbuf[wi[0] % 3]; wi[0] += 1
                        S.dma(wb[:, :, 0:n * M], winb_d[:, :, c0 + g * M:c0 + (g + n) * M])
                        p_ = pfA()
                        for j in range(n):
                            for k in range(8):
                                S.mm(p_[0:M, j * 128:j * 128 + Tn], wb[:, k, j * M:(j + 1) * M], nT[:, k, 0:Tn],
                                     start=(k == 0), stop=(k == 7))
                        dst_fn(g, n, p_[0:M, 0:n * 128].rearrange("p (j t) -> p j t", j=n))
                        g += n
                qTv = v64v(sc[0]); kTv = v64v(sc[1])
                proj(0, 64, 4, lambda g, n, v: S.cp('act', qTv[:, g:g + n, 0:Tn], v[:, :, 0:Tn]))
                proj(256, 64, 4, lambda g, n, v: S.cp('act', kTv[:, g:g + n, 0:Tn], v[:, :, 0:Tn]))
                yield
                proj(512, 128, 4, lambda g, n, v: S.cp('act', vT[:, g:g + n, 0:Tn], v[:, :, 0:Tn]))
                proj(1024, 128, 4, lambda g, n, v: S.cp('dve', ggT[:, g:g + n, 0:Tn], v[:, :, 0:Tn]))
                proj(1536, 16, 1, lambda g, n, v: S.cp('act', glT[:, 0:Tn], v[:, 0, 0:Tn]))
                yield
                p_ = pfA()
                S.mm(p_[0:Tn, 0:256], glT[0:16, 0:Tn], gw2f[:, :], start=True, stop=False)
                S.mm(p_[0:Tn, 0:256], onesf[0:1, 0:Tn], gbf[0:1, :], start=False, stop=True)
                S.act(sc[2][0:Tn, 0:256], p_[0:Tn, 0:256], AF.Exp, scale=-1.0)
                S.act(sc[2][0:Tn, 256:512], sc[2][0:Tn, 0:256], AF.Ln, bias=1.0)
                lgt = sc[2][:, 256:512]
                EQ = v64v(sc[3]); EK = v64v(sc[4]); ED = v64v(sc[5])
                for hp in range(2):
                    p_ = pfA()
                    for hh in range(2):
                        h = hp * 2 + hh
                        S.mm(p_[0:64, hh * 256:hh * 256 + Tn], lgt[0:Tn, h * 64:(h + 1) * 64], mi[0:Tn, 0:Tn])
                        S.mm(p_[0:64, hh * 256 + 128:hh * 256 + 128 + Tn], lgt[0:Tn, h * 64:(h + 1) * 64], mls[0:Tn, 0:Tn])
                    cs = p_[0:64, :].rearrange("p (h a t) -> p h a t", h=2, a=2)
                    hs = slice(hp * 2, hp * 2 + 2)
                    S.act(EQ[:, hs, 0:Tn], cs[:, :, 0, 0:Tn], AF.Exp, scale=-1.0 / 16)
                    S.act(EK[:, hs, 0:Tn], cs[:, :, 0, 0:Tn], AF.Exp, scale=1.0 / 16)
                    S.act(ED[:, hs, 0:Tn], cs[:, :, 1, 0:Tn], AF.Exp, scale=-1.0 / 16)
                S.stt('dve', qs[:, :, 0:Tn], qTv[:, :, 0:Tn], 0.125, EQ[:, :, 0:Tn], ALU.mult, ALU.mult)
                S.tt('pool', ks[:, :, 0:Tn], kTv[:, :, 0:Tn], EK[:, :, 0:Tn], ALU.mult)
                S.tt('dve', khT[:, :, 0:Tn], kTv[:, :, 0:Tn], ED[:, :, 0:Tn], ALU.mult)
                if sample:
                    S.cp('pool', gam[:, :, :], EQ[:, :, 0:64].rearrange("p h (s t) -> p h s t", t=4)[:, :, :, 3])
                else:
                    S.cp('pool', gam[:, :, 0:1], EQ[:, :, Tn - 1:Tn])
                yield

                bi = [0]

                def rw_evac128(g0, n, v, dstf):
                    t4 = v128v(sc[6 + bi[0] % 2]); bi[0] += 1
                    dst = dstf[:, :, 0:Tn]
                    cur = v[:, :, 0:Tn]
                    mu_ = mu128[:, g0:g0 + n]; omu_ = omu128[:, g0:g0 + n]
                    S.tt('dve', dst, cur, bc(omu_, [128, n, Tn], 2), ALU.mult)
                    if sample:
                        c4 = cur.rearrange("p g (s t) -> p g s t", t=4)
                        t44 = t4[:, 0:n, 0:Tn].rearrange("p g (s t) -> p g s t", t=4)
                        S.tt('dve', t44[:, :, :, 1:4], c4[:, :, :, 0:3],
                             mu_.unsqueeze(2).unsqueeze(3).to_broadcast([128, n, 16, 3]), ALU.mult)
                        S.tt('pool', t44[:, :, :, 0], shT128[:, g0:g0 + n, :], bc(mu_, [128, n, 16], 2), ALU.mult)
                    else:
                        S.tt('dve', t4[:, 0:n, 1:Tn], v[:, :, 0:Tn - 1], bc(mu_, [128, n, Tn - 1], 2), ALU.mult)
                        S.tt('pool', t4[:, 0:n, 0:1], c128_old[:, g0:g0 + n].unsqueeze(2), mu_.unsqueeze(2), ALU.mult)
                        S.cp('dve', c128_new[:, g0:g0 + n].unsqueeze(2), v[:, :, Tn - 1:Tn])
                    S.tt('pool', dst, dst, t4[:, 0:n, 0:Tn], ALU.add)

                def rw_evac(g0, n, v):
                    t4 = v64v(sc[6 + bi[0] % 2]); bi[0] += 1
                    dst = vTb[:, g0 - 16:g0 - 16 + n, 0:Tn]
                    cur = v[:, :, 0:Tn]
                    mu_ = mu64[:, g0:g0 + n]; omu_ = omu64[:, g0:g0 + n]
                    S.tt('dve', dst, cur, bc(omu_, [64, n, Tn], 2), ALU.mult)
                    if sample:
                        c4 = cur.rearrange("p g (s t) -> p g s t", t=4)
                        t44 = t4[:, 0:n, 0:Tn].rearrange("p g (s t) -> p g s t", t=4)
                        S.tt('dve', t44[:, :, :, 1:4], c4[:, :, :, 0:3],
                             mu_.unsqueeze(2).unsqueeze(3).to_broadcast([64, n, 16, 3]), ALU.mult)
                        S.tt('pool', t44[:, :, :, 0], shT[:, g0:g0 + n, :], bc(mu_, [64, n, 16], 2), ALU.mult)
                    else:
                        S.tt('dve', t4[:, 0:n, 1:Tn], v[:, :, 0:Tn - 1], bc(mu_, [64, n, Tn - 1], 2), ALU.mult)
                        S.tt('pool', t4[:, 0:n, 0:1], c_old[:, g0:g0 + n].unsqueeze(2), mu_.unsqueeze(2), ALU.mult)
                        S.cp('dve', c_new[:, g0:g0 + n].unsqueeze(2), v[:, :, Tn - 1:Tn])
                    S.tt('pool', dst, dst, t4[:, 0:n, 0:Tn], ALU.add)
                proj(GLA_COLS, 128, 4, lambda g, n, v: rw_evac128(g, n, v, xr[:, g:g + n, :]))
                yield
                proj(GLA_COLS + 512, 128, 4, lambda g, n, v: rw_evac128(4 + g, n, v, xk[:, g:g + n, :]))
                yield
                proj(GLA_COLS + 1024, 64, 8, lambda g, n, v: rw_evac(g + 16, n, v))
                proj(GLA_COLS + 1536, 128, 1, lambda g, n, v: rw_evac128(8, 1, v, xwa[:, 0:1, :]))
                yield

                def rwg_evac(g0, n, v):
                    cur = v[:, 0, 0:Tn]
                    S.ts('dve', xmG[:, 0:Tn], cur, omu_g[:, 0:1], None, ALU.mult)
                    if sample:
                        c3 = cur.rearrange("p (s t) -> p s t", t=4)
                        t3 = t4g[:, 0:Tn].rearrange("p (s t) -> p s t", t=4)
                        S.ts('dve', t3[:, :, 1:4], c3[:, :, 0:3], mu_g, None, ALU.mult)
                        S.ts('pool', t3[:, :, 0], shTG[:, :], mu_g, None, ALU.mult)
                    else:
                        S.ts('dve', t4g[:, 1:Tn], v[:, 0, 0:Tn - 1], mu_g, None, ALU.mult)
                        S.ts('pool', t4g[:, 0:1], cg_old[:, 0:1], mu_g, None, ALU.mult)
                        S.cp('dve', cg_new[:, 0:1], v[:, 0, Tn - 1:Tn])
                    S.tt('pool', xmG[:, 0:Tn], xmG[:, 0:Tn], t4g[:, 0:Tn], ALU.add)
                proj(GLA_COLS + 1664, 128, 1, rwg_evac)
                rT = xr[:, :, 0:Tn]; kraw = xk[:, :, 0:Tn]
                S.act(twl[:, 0:Tn], xwa[0:64, 0, 0:Tn], AF.Exp, scale=-2.0)
                S.act(twl[:, 0:Tn], twl[:, 0:Tn], AF.Ln, bias=1.0)
                S.act(twl[:, 0:Tn], twl[:, 0:Tn], AF.Exp, scale=-1.0)
                S.ts('dve', twlb[:, 0:Tn], twl[:, 0:Tn], 2.0, -1.0, ALU.mult, ALU.add)
                S.cp('pool', alb128[64:128, 0:Tn], xwa[64:128, 0, 0:Tn])
                S.act(sgl[:, 0:Tn], xmG[:, 0:Tn], AF.Exp, scale=-1.0)
                S.act(sgl[:, 0:Tn], sgl[:, 0:Tn], AF.Ln, bias=1.0)
                S.act(sglb[:, 0:Tn], sgl[:, 0:Tn], AF.Exp, scale=-1.0)
                p_ = pfA()
                S.mm(p_[0:Tn, :], twlb[:, 0:Tn], w2b[:, :], start=True, stop=False)
                S.mm(p_[0:Tn, :], onesf[0:1, 0:Tn], w0r[0:1, :], start=False, stop=True)
                S.act(slw[0:Tn, :], p_[0:Tn, :], AF.Exp, scale=-1.0)
                S.act(slw[0:Tn, :], slw[0:Tn, :], AF.Ln, bias=1.0)
                S.act(slw[0:Tn, :], slw[0:Tn, :], AF.Exp, scale=-1.0)
                yield
                ER = v128v(sc[0])[:, :, 0:Tn]; EB = v128v(sc[1])[:, :, 0:Tn]; EA = v128v(sc[2])[:, :, 0:Tn]
                aT = v128v(sc[3])[:, :, 0:Tn]; kk = v128v(sc[4])[:, :, 0:Tn]; s5 = v128v(sc[5])[:, :, 0:Tn]
                kp = v128v(sc[6])[:, :, 0:Tn]
                for hp in range(2):
                    p_ = pfA()
                    for cc in range(2):
                        c = hp * 2 + cc
                        S.mm(p_[:, cc * 256:cc * 256 + Tn], slw[0:Tn, c * 128:(c + 1) * 128], mi[0:Tn, 0:Tn])
                        S.mm(p_[:, cc * 256 + 128:cc * 256 + 128 + Tn], slw[0:Tn, c * 128:(c + 1) * 128], mus[0:Tn, 0:Tn])
                    cw = p_[:, :].rearrange("p (h a t) -> p h a t", h=2, a=2)
                    h2 = slice(hp * 2, hp * 2 + 2)
                    S.act(v128v(sc[0])[:, h2, 0:Tn], cw[:, :, 0, 0:Tn], AF.Exp, scale=DECAY)
                    S.act(v128v(sc[1])[:, h2, 0:Tn], cw[:, :, 0, 0:Tn], AF.Exp, scale=-DECAY)
                    S.act(v128v(sc[2])[:, h2, 0:Tn], cw[:, :, 1, 0:Tn], AF.Exp, scale=DECAY)
                p_ = pfA()
                ncol = 16 if sample else 1
                selr = (blk_i[0:64, 0:64].rearrange("p (s t) -> p s t", t=4)[:, :, 3]) if sample else onesf[0:Tn, 0:1]
                for h in range(8):
                    S.mm(p_[0:64, h * 16:h * 16 + ncol], slw[0:Tn, h * 64:(h + 1) * 64], selr)
                S.act(WCs[:, :, 0:ncol], p_[0:64, 0:128].rearrange("p (h s) -> p h s", h=8)[:, :, 0:ncol], AF.Exp, scale=DECAY)
                p_ = pfA()
                for c in range(4):
                    S.mm(p_[:, c * 128:c * 128 + Tn], a2b128[64:128, c * 128:(c + 1) * 128], alb128[64:128, 0:Tn])
                pa = p_[:, :].rearrange("p (h t) -> p h t", h=4)[:, :, 0:Tn]
                S.tt('dve', aT, pa, bc(a0c128, [128, 4, Tn], 2), ALU.add)
                S.act(aT, aT, AF.Exp, scale=-1.0)
                S.act(aT, aT, AF.Ln, bias=1.0)
                S.act(aT, aT, AF.Exp, scale=-1.0)
                yield
                S.tt('pool', kk, kraw, bc(kkw128, [128, 4, Tn], 2), ALU.mult)
                S.tt('pool', s5, kk, kk, ALU.mult)
                p_ = pfA()
                for c in range(4):
                    S.mm(p_[:, c * 128:c * 128 + Tn], blkones[:, :], v128v(sc[5])[:, c, 0:Tn])
                pk = p_[:, :].rearrange("p (h t) -> p h t", h=4)[:, :, 0:Tn]
                S.act(s5, pk, AF.Ln, bias=1e-24)
                S.act(s5, s5, AF.Exp, scale=-0.5)
                S.tt('dve', kk, kk, s5, ALU.mult)
                S.stt('dve', s5, aT, -1.0, bc(kac128, [128, 4, Tn], 2), ALU.add, ALU.mult)
                S.stt('dve', kp, s5, 1.0, kraw, ALU.add, ALU.mult)
                S.tt('pool', s5, rT, kp, ALU.mult)
                S.tt('pool', rkr[:, :, 0:Tn], s5, bc(rkc128, [128, 4, Tn], 2), ALU.mult)
                yield
                S.stt('dve', ARt[:, :, 0, 0:Tn], kk, -1.0, EA, ALU.mult, ALU.mult)
                S.tt('pool', ARt[:, :, 1, 0:Tn], rT, ER, ALU.mult)
                S.tt('dve', s5, kk, aT, ALU.mult)
                S.tt('dve', BtT[:, :, 0:Tn], s5, EB, ALU.mult)
                S.tt('pool', KtT[:, :, 0:Tn], kp, EB, ALU.mult)
                yield
                if slot == 15 or sample:
                    lo = 127 if slot == 15 else 0
                    M = 1 if slot == 15 else 64
                    for cb in range(7):
                        c0 = GLA_COLS + cb * 256
                        wb = wbuf[wi[0] % 3]; wi[0] += 1
                        S.dma(wb[:, :, :], winb_d[:, :, c0:c0 + 256])
                        p_ = pfA()
                        for k in range(8):
                            S.mm(p_[0:M, 0:256], nT[:, k, lo:lo + M], wb[:, k, :], start=(k == 0), stop=(k == 7))
                        S.cp('act', shrow[0:M, cb * 256:(cb + 1) * 256], p_[0:M, 0:256])
                    if slot == 15:
                        S.dma(shp_d, shrow[0:1, :], eng='act')
                    else:
                        for s_ in range(16):
                            S.dma(shs_d[s_:s_ + 1, :], shrow[4 * s_ + 3:4 * s_ + 4, :])
                yield

            HP = lambda h: slice(64 * (h % 2), 64 * (h % 2) + 64)

            def st2(slot, H):
                sample, Tn, tok0 = tile_info(slot)
                vT = H['vT']; ggT = H['ggT']; qs = H['qs']; ks = H['ks']; khT = H['khT']; gam = H['gam']; WCs = H['WCs']
                vTb = H['vTb']; ARt = H['ARt']; BtT = H['BtT']; KtT = H['KtT']; rkr = H['rkr']; sglb = H['sglb']
                chunks = [(s * 4, 4, s) for s in range(16)] if sample else [(0, Tn, None)]
                for ci, (o, Cn, seq) in enumerate(chunks):
                    tsl = slice(o, o + Cn)
                    if sample:
                        S.dma(Sg[:], sg_d[seq].rearrange("h k v -> k h v"))
                        S.cp('pool', Sgb[:], Sg[:])
                        srin = s2[3][0:64, :].rearrange("p (h j) -> p h j", h=8)
                        S.dma(srin, sr_d[seq].rearrange("h i j -> i h j"))
                        p_ = pfB()
                        for h in range(8):
                            S.tr(p_[0:64, h * 64:(h + 1) * 64], srin[:, h, :], identf[0:64, 0:64])
                        S.cp('dve', Sr[:], p_[0:64, :].rearrange("p (h i) -> p h i", h=8))
                        S.cp('act', Srb[:], p_[0:64, :].rearrange("p (h i) -> p h i", h=8))
                    p_ = psB[1]
                    for h in range(4):
                        S.tr(p_[0:Cn, h * 128:(h + 1) * 128], vT[:, h, tsl], identb[:, :])
                        S.tr(p_[0:Cn, 512 + h * 64:512 + (h + 1) * 64], khT[:, h, tsl], identb[0:64, 0:64])
                    S.cp('act', Vg[0:Cn, :], p_[0:Cn, 0:768])
                    p_ = pfB()
                    for h in range(4):
                        S.mm(p_[0:Cn, h * 128:h * 128 + Cn], ks[:, h, tsl], qs[:, h, tsl])
                    S.tt('dve', scT[0:Cn, :, 0:Cn], p_[0:Cn, :].rearrange("p (h t) -> p h t", h=4)[:, :, 0:Cn],
                         bc(triu_i[0:Cn, 0:Cn], [Cn, 4, Cn], 1), ALU.mult)
                    p_ = pfB()
                    for h in range(4):
                        S.mm(p_[:, h * 128:h * 128 + Cn], Vg[0:Cn, h * 128:(h + 1) * 128], scT[0:Cn, h, 0:Cn], start=True, stop=False)
                        S.mm(p_[:, h * 128:h * 128 + Cn], Sgb[:, h, :], qs[:, h, tsl], start=False, stop=True)
                    S.cp('act', ogT[:, :, tsl], p_[:, :].rearrange("p (h t) -> p h t", h=4)[:, :, 0:Cn])
                    p_ = pfB()
                    for h in range(4):
                        S.mm(p_[0:64, h * 128:(h + 1) * 128], Vg[0:Cn, 512 + h * 64:512 + (h + 1) * 64], Vg[0:Cn, h * 128:(h + 1) * 128])
                    sgt = v64v(s2[0])
                    S.tt('pool', sgt, Sg[:], bc(gam[:, :, ci], [64, 4, 128], 2), ALU.mult)
                    S.tt('dve', Sg[:], sgt, p_[0:64, :].rearrange("p (h v) -> p h v", h=4), ALU.add)
                    S.cp('pool', Sgb[:], Sg[:])
                    if sample:
                        S.dma(glas_d[seq].rearrange("h k v -> k h v"), Sg[:])
                    yield
                    p_ = psB[1]
                    for c in range(4):
                        S.tr(p_[0:Cn, c * 128:(c + 1) * 128], ARt[:, c, 0, tsl], identb[:, :])
                        S.tr(p_[0:Cn, 512 + c * 128:512 + (c + 1) * 128], BtT[:, c, tsl], identb[:, :])
                    pv = p_[0:Cn, :].rearrange("p (a h j) -> p a h j", a=2, h=8)
                    S.cp('act', rhsPQ[0:Cn, :, 0, :], pv[:, 0, :, :])
                    S.cp('dve', tokB[0:Cn, :, :], pv[:, 1, :, :])
                    p_ = psB[1]
                    for c in range(4):
                        S.tr(p_[0:Cn, c * 128:(c + 1) * 128], KtT[:, c, tsl], identb[:, :])
                    for h in range(8):
                        S.tr(p_[0:Cn, 512 + h * 64:512 + (h + 1) * 64], vTb[:, h, tsl], identb[0:64, 0:64])
                    S.cp('act', tokKV[0:Cn, :, :, :], p_[0:Cn, :].rearrange("p (a h j) -> p a h j", a=2, h=8))
                    small = Cn <= 16
                    MhV = (lambda h: MhS[:, h]) if small else (lambda h: Mh[h])
                    if small:
                        for par in range(2):
                            p_ = pfB()
                            pm8 = p_[0:Cn, 0:256].rearrange("p (h q t) -> p h q t", h=4, q=4)
                            for hi in range(4):
                                h = 2 * hi + par
                                for a_ in range(2):
                                    S.mm(pm8[:, hi, a_, 0:Cn], BtT[HP(h), h // 2, tsl], ARt[HP(h), h // 2, a_, tsl])
                                    S.mm(pm8[:, hi, 2 + a_, 0:Cn], KtT[HP(h), h // 2, tsl], ARt[HP(h), h // 2, a_, tsl])
                            dstv = MhS[0:Cn, :, :, 0:Cn].rearrange("p (hi two) q t -> p hi two q t", two=2)[:, :, par]
                            S.tt('dve', dstv, pm8[:, :, :, 0:Cn], bc(m4[0:Cn, :, 0:Cn], [Cn, 4, 4, Cn], 1), ALU.mult)
                    else:
                        for h in range(8):
                            p_ = pfB()
                            pm = p_[0:Cn, :].rearrange("p (q t) -> p q t", q=4)
                            for a_ in range(2):
                                S.mm(pm[:, a_, 0:Cn], BtT[HP(h), h // 2, tsl], ARt[HP(h), h // 2, a_, tsl])
                                S.mm(pm[:, 2 + a_, 0:Cn], KtT[HP(h), h // 2, tsl], ARt[HP(h), h // 2, a_, tsl])
                            S.tt('dve', Mh[h][0:Cn, :, 0:Cn], pm[:, :, 0:Cn], m4[0:Cn, :, 0:Cn], ALU.mult)
                    yield
                    nlev = {4: 1, 16: 3, 128: 6}[Cn]
                    for half in range(2):
                        p_ = psB[1]
                        for hh in range(4):
                            h = half * 4 + hh
                            S.tr(p_[0:Cn, hh * 128:hh * 128 + Cn], MhV(h)[0:Cn, 0, 0:Cn], identb[0:Cn, 0:Cn])
                        S.cp('dve', YZ[half][0:Cn, :, 1, 0:Cn], p_[0:Cn, 0:512].rearrange("p (h s) -> p h s", h=4)[:, :, 0:Cn])
                        if small:
                            mh0 = MhS[0:Cn, 4 * half:4 * half + 4, 0, 0:Cn]
                            S.cp('pool', YZ[half][0:Cn, :, 0, 0:Cn], mh0)
                            S.tt('pool', TT[half][0:Cn, :, 0:Cn], mh0, bc(identb[0:Cn, 0:Cn], [Cn, 4, Cn], 1), ALU.add)
                        else:
                            for hh in range(4):
                                h = half * 4 + hh
                                S.cp('pool', YZ[half][0:Cn, hh, 0, 0:Cn], Mh[h][0:Cn, 0, 0:Cn])
                                S.tt('pool', TT[half][0:Cn, hh, 0:Cn], Mh[h][0:Cn, 0, 0:Cn], identb[0:Cn, 0:Cn], ALU.add)
                    yield
                    for lvl in range(nlev):
                        lastl = (lvl == nlev - 1)
                        for half in range(2):
                            yz = YZ[half]
                            if half == 0:
                                pyz = psW[0:Cn, :].rearrange("p (h a t) -> p h a t", h=4, a=2)
                                pY = pyz[:, :, 0, :]; pZ = pyz[:, :, 1, :]
                            else:
                                pY = psF[2][0:Cn, :].rearrange("p (h t) -> p h t", h=4)
                                pZ = psF[3][0:Cn, :].rearrange("p (h t) -> p h t", h=4)
                            for hh in range(4):
                                if not lastl:
                                    S.mm(pY[:, hh, 0:Cn], yz[0:Cn, hh, 1, 0:Cn], yz[0:Cn, hh, 0, 0:Cn])
                                S.mm(pZ[:, hh, 0:Cn], yz[0:Cn, hh, 0, 0:Cn], yz[0:Cn, hh, 1, 0:Cn])
                        for half in range(2):
                            yz = YZ[half]
                            if half == 0:
                                pyz = psW[0:Cn, :].rearrange("p (h a t) -> p h a t", h=4, a=2)
                                if lastl:
                                    S.cp('act', yz[0:Cn, :, 1, 0:Cn], pyz[:, :, 1, 0:Cn])
                                else:
                                    S.cp('act', yz[0:Cn, :, :, 0:Cn], pyz[:, :, :, 0:Cn])
                            else:
                                pY = psF[2][0:Cn, :].rearrange("p (h t) -> p h t", h=4)
                                pZ = psF[3][0:Cn, :].rearrange("p (h t) -> p h t", h=4)
                                S.cp('dve', yz[0:Cn, :, 1, 0:Cn], pZ[:, :, 0:Cn])
                                if not lastl:
                                    S.cp('act', yz[0:Cn, :, 0, 0:Cn], pY[:, :, 0:Cn])
                        for half in range(2):
                            yz = YZ[half]
                            p_ = psB1f
                            for hh in range(4):
                                S.mm(p_[0:Cn, hh * 128:hh * 128 + Cn], yz[0:Cn, hh, 1, 0:Cn], TT[half][0:Cn, hh, 0:Cn])
                            S.tt('dve', TT[half][0:Cn, :, 0:Cn], TT[half][0:Cn, :, 0:Cn],
                                 p_[0:Cn, :].rearrange("p (h t) -> p h t", h=4)[:, :, 0:Cn], ALU.add)
                        yield
                    p_ = pfB()
                    for h in range(8):
                        S.mm(p_[0:Cn, h * 64:(h + 1) * 64], MhV(h)[0:Cn, 2, 0:Cn], tokKV[0:Cn, 1, h, :])
                    S.cp('act', rhsPQ[0:Cn, :, 1, :], p_[0:Cn, :].rearrange("p (h i) -> p h i", h=8))
                    for h in range(8):
                        S.mm(psW[0:Cn, h * 128:(h + 1) * 128], TT[h // 4][0:Cn, h % 4, 0:Cn], rhsPQ[0:Cn, h, :, :].rearrange("p a j -> p (a j)"))
                    S.cp('act', PQ[0:Cn, :, :, :], psW[0:Cn, :].rearrange("p (h a j) -> p h a j", h=8, a=2))
                    yield
                    p_ = pfB()
                    for h in range(8):
                        S.mm(p_[0:64, h * 64:(h + 1) * 64], PQ[0:Cn, h, 0, :], tokB[0:Cn, h, :])
                    S.tt('dve', GT[:], p_[0:64, :].rearrange("p (h j) -> p h j", h=8), bc(identf[0:64, 0:64], [64, 8, 64], 1), ALU.add)
                    p_ = pfB()
                    for h in range(8):
                        S.mm(p_[0:64, h * 64:(h + 1) * 64], tokB[0:Cn, h, :], PQ[0:Cn, h, 1, :], start=True, stop=False)
                        S.mm(p_[0:64, h * 64:(h + 1) * 64], tokKV[0:Cn, 0, h, :], tokKV[0:Cn, 1, h, :], start=False, stop=True)
                    wcb = bc(WCs[:, :, ci], [64, 8, 64], 2)
                    S.tt('dve', HW[:], p_[0:64, :].rearrange("p (h i) -> p h i", h=8), wcb, ALU.mult)
                    for h in range(8):
                        S.mm(psW[0:64, h * 128:h * 128 + Cn], PQ[0:Cn, h, 0, :], MhV(h)[0:Cn, 1, 0:Cn], start=True, stop=False)
                        S.mm(psW[0:64, h * 128:h * 128 + Cn], identb[:, HP(h)], ARt[:, h // 2, 1, tsl], start=False, stop=True)
                    S.cp('dve', ATb[:, :, 0:Cn], psW[0:64, :].rearrange("p (h t) -> p h t", h=8)[:, :, 0:Cn])
                    yield
                    for h in range(8):
                        o_ = psW[0:64, h * 128:h * 128 + Cn]
                        S.mm(o_, PQ[0:Cn, h, 1, :], MhV(h)[0:Cn, 1, 0:Cn], start=True, stop=False)
                        S.mm(o_, tokKV[0:Cn, 1, h, :], MhV(h)[0:Cn, 3, 0:Cn], start=False, stop=False)
                        S.mm(o_, Srb[:, h, :], ATb[:, h, 0:Cn], start=False, stop=True)
                    S.cp('act', yT[:, :, tsl], psW[0:64, :].rearrange("p (h t) -> p h t", h=8)[:, :, 0:Cn])
                    p_ = pfB()
                    for h in range(8):
                        S.mm(p_[0:64, h * 64:(h + 1) * 64], GT[:, h, :], Sr[:, h, :])
                    srt = s2[1][0:64, :].rearrange("p (h i) -> p h i", h=8)
                    S.tt('dve', srt, p_[0:64, :].rearrange("p (h i) -> p h i", h=8), wcb, ALU.mult)
                    S.tt('pool', Sr[:], srt, HW[:], ALU.add)
                    S.cp('pool', Srb[:], Sr[:])
                    if sample or slot == 15:
                        p_ = pfB()
                        for h in range(8):
                            S.tr(p_[0:64, h * 64:(h + 1) * 64], Sr[:, h, :], identf[0:64, 0:64])
                        srout = s2[2][0:64, :].rearrange("p (h j) -> p h j", h=8)
                        S.cp('act', srout, p_[0:64, :].rearrange("p (h j) -> p h j", h=8))
                        dst = rws_d[seq] if sample else rwp_d
                        S.dma(dst.rearrange("h i j -> i h j"), srout, eng=('sp' if sample else 'act'))
                    yield
                if slot == 15:
                    S.dma(glap_d.rearrange("h k v -> k h v"), Sg[:], eng='act')
                if slot < 0:
                    return
                F = 4 * Tn
                og = ogT[:, :, 0:Tn]
                f3 = lambda t: t[:, 0:F].rearrange("p (h t) -> p h t", h=4)
                S.tt('pool', f3(s2[0]), og, og, ALU.mult)
                p_ = pfB()
                S.mm(p_[:, 0:F], onesf[:, :], s2[0][:, 0:F])
                S.act(s2[1][:, 0:F], p_[:, 0:F], AF.Ln, scale=1.0 / 128, bias=1e-6)
                S.act(s2[1][:, 0:F], s2[1][:, 0:F], AF.Exp, scale=-0.5)
                gg3 = ggT[:, :, 0:Tn]
                S.act(f3(s2[2]), gg3, AF.Exp, scale=-1.0)
                S.act(s2[2][:, 0:F], s2[2][:, 0:F], AF.Ln, bias=1.0)
                S.act(s2[2][:, 0:F], s2[2][:, 0:F], AF.Exp, scale=-1.0)
                S.tt('dve', f3(s2[2]), f3(s2[2]), gg3, ALU.mult)
                S.tt('dve', f3(s2[0]), og, f3(s2[1]), ALU.mult)
                S.stt('dve', actG[:, :, tok0:tok0 + Tn], f3(s2[0]), gnorm, f3(s2[2]), ALU.mult, ALU.mult)
                yield
                for half in range(2):
                    hs = slice(half * 4, half * 4 + 4)
                    ya = v64v(s2[3])[:, :, 0:Tn]; yb = v64v(s2[4])[:, :, 0:Tn]

                    def onesmm(lhs, srcf, rk=False):
                        p_ = pfB()
                        for hh in range(4):
                            if rk:
                                h = half * 4 + hh
                                S.mm(p_[0:64, hh * 128:hh * 128 + Tn], blkonesb[:, HP(h)], rkr[:, h // 2, 0:Tn])
                            else:
                                S.mm(p_[0:64, hh * 128:hh * 128 + Tn], lhs, srcf(hh))
                        return p_[0:64, :].rearrange("p (h t) -> p h t", h=4)[:, :, 0:Tn]
                    pm_ = onesmm(ones64[0:64, 0:64], lambda hh: yT[:, half * 4 + hh, 0:Tn])
                    S.tt('dve', ya, yT[:, hs, 0:Tn], pm_, ALU.subtract)
                    S.tt('pool', yb, ya, ya, ALU.mult)
                    pm_ = onesmm(ones64[0:64, 0:64], lambda hh: v64v(s2[4])[:, hh, 0:Tn])
                    S.act(yb, pm_, AF.Ln, bias=64e-5)
                    S.act(yb, yb, AF.Exp, scale=-0.5)
                    S.tt('dve', ya, ya, yb, ALU.mult)
                    S.tt('pool', ya, ya, bc(lnw[:, hs], [64, 4, Tn], 2), ALU.mult)
                    S.tt('pool', ya, ya, bc(lnb[:, hs], [64, 4, Tn], 2), ALU.add)
                    pm_ = onesmm(None, None, rk=True)
                    S.tt('dve', yb, pm_, vTb[:, hs, 0:Tn], ALU.mult)
                    S.tt('pool', ya, ya, yb, ALU.add)
                    p_ = pfB()
                    for hh in range(4):
                        h = half * 4 + hh
                        S.mm(p_[0:64, hh * 128:hh * 128 + Tn], g2b[:, h * 64:(h + 1) * 64], sglb[:, 0:Tn])
                    pg = p_[0:64, :].rearrange("p (c two t) -> p c two t", c=2, two=2)[:, :, :, 0:Tn]
                    ya4 = s2[3][0:64, :].rearrange("p (c two t) -> p c two t", c=2, two=2)[:, :, :, 0:Tn]
                    cs_ = slice(half * 2, half * 2 + 2)
                    S.tt('dve', actR[0:64, cs_, tok0:tok0 + Tn], ya4[:, :, 0, :], pg[:, :, 0, :], ALU.mult)
                    S.tt('dve', oRo[:, cs_, 0:Tn], ya4[:, :, 1, :], pg[:, :, 1, :], ALU.mult)
                    yield
                S.dma(actR[64:128, :, tok0:tok0 + Tn], oRo[:, :, 0:Tn], eng=('sp' if sample else 'act'))
                yield

            def run(g):
                for _ in g:
                    pass

            def interleave(ga, gb):
                live = [ga, gb]
                while live:
                    for g in list(live):
                        try:
                            next(g)
                        except StopIteration:
                            live.remove(g)
            slots = ([-1] + list(range(17)))[:K_NT]
            if slots:
                run(st1(slots[0], HB[0]))
            for i, slot in enumerate(slots):
                g2_ = st2(slot, HB[i % 2])
                if i + 1 < len(slots):
                    interleave(g2_, st1(slots[i + 1], HB[(i + 1) % 2]))
                else:
                    run(g2_)
            S.barrier()
            S.emit()

        xres = sbG("xres", [128, NSLOT, D])
        comb = sbG("comb", [128, NSLOT, NE])
        with ExitStack() as p2:
            sb = lambda n, s, d=F32: p2.enter_context(nc.sbuf_tensor("s_" + n, list(s), d))
            ps = lambda n, s, d=F32: p2.enter_context(nc.psum_tensor(n, list(s), d))
            psM = [ps("psM%d" % i, [128, 512]) for i in range(2)]
            psXs = [ps("psX%d" % i, [128, 1024]) for i in range(2)]
            psRs = [ps("psR%d" % i, [128, 512]) for i in range(2)]
            wo = sb("wo", [128, 8, D], BF16)
            stgo = [sb("stgo%d" % i, [128, 2, D]) for i in range(2)]
            for q in range(4):
                S.dma(stgo[q % 2][:], wout_d[:, 2 * q:2 * q + 2, :])
                S.cp(['pool', 'dve'][q % 2], wo[:, 2 * q:2 * q + 2, :], stgo[q % 2][:])
            wr = sb("wr", [128, 8, 36]); rb = sb("rb", [1, 36])
            S.dma(wr[:], wr_d); S.dma(rb[:], rb_d)
            B2 = []
            for i in range(2):
                B2.append(dict(junk2=sb("junk2_%d" % i, [128, D], BF16), xn=sb("xn%d" % i, [128, D]), xnT=sb("xnT%d" % i, [128, 8, 128]),
                               st2=sb("st2_%d" % i, [128, 4]), lg=sb("lg%d" % i, [128, 36]), r1=sb("r1_%d" % i, [128, 8]),
                               r2=sb("r2_%d" % i, [128, 8]), els=sb("els%d" % i, [128, 32]), mk1=sb("mk1_%d" % i, [128, 32]),
                               mk2=sb("mk2_%d" % i, [128, 32]), gm=sb("gm%d" % i, [128, 4])))
            def slot1b(slot):
                Sched.keymap = {'s_xres': ('xres', slot), 's_actG': ('actG', slot), 's_actR': ('actR', slot), 's_comb': ('comb', slot)}
                b_ = B2[slot % 2]
                junk2 = b_['junk2']; xn = b_['xn']; xnT = b_['xnT']; st2 = b_['st2']; lg = b_['lg']; r1 = b_['r1']
                r2 = b_['r2']; els = b_['els']; mk1 = b_['mk1']; mk2 = b_['mk2']; gm = b_['gm']
                psX = psXs[slot % 2]; psR = psRs[slot % 2]
                sample = (slot == 16)
                Tn = 64 if sample else 128
                tok0 = slot * 128
                src = xs_d if sample else x_d[slot * 128:(slot + 1) * 128, :]
                S.dma(xres[0:Tn, slot, :], src)
                for hf in range(2):
                    p_ = psM[hf]
                    for k in range(8):
                        a_ = actG if k < 4 else actR
                        S.mm(p_[0:Tn, :], a_[:, k % 4, tok0:tok0 + Tn], wo[:, k, hf * 512:(hf + 1) * 512], start=(k == 0), stop=(k == 7))
                    S.tt('dve', xres[0:Tn, slot, hf * 512:(hf + 1) * 512], xres[0:Tn, slot, hf * 512:(hf + 1) * 512], p_[0:Tn, :], ALU.add)
                S.memset('pool', st2[0:Tn, 0:1], 0.0)
                S.act(junk2[0:Tn, :], xres[0:Tn, slot, :], AF.Square, accum=st2[0:Tn, 0:1])
                S.act(st2[0:Tn, 1:2], st2[0:Tn, 0:1], AF.Ln, scale=1.0 / D, bias=1e-6)
                S.act(st2[0:Tn, 2:3], st2[0:Tn, 1:2], AF.Exp, scale=-0.5)
                S.act(xn[0:Tn, :], xres[0:Tn, slot, :], AF.Copy, scale=st2[0:Tn, 2:3])
                yield
                Sched.keymap = {'s_xres': ('xres', slot), 's_actG': ('actG', slot), 's_actR': ('actR', slot), 's_comb': ('comb', slot)}
                for k in range(8):
                    S.tr(psX[:, k * 128:k * 128 + Tn], xn[0:Tn, k * 128:(k + 1) * 128], identf[0:Tn, 0:Tn])
                S.tt('dve', xnT[:, :, 0:Tn], psX[:, :].rearrange("p (k t) -> p k t", k=8)[:, :, 0:Tn], bc(g_ffn, [128, 8, Tn], 2), ALU.mult)
                for k in range(8):
                    S.mm(psR[0:Tn, 0:36], xnT[:, k, 0:Tn], wr[:, k, :], start=(k == 0), stop=False)
                S.mm(psR[0:Tn, 0:36], onesf[0:1, 0:Tn], rb[0:1, :], start=False, stop=True)
                S.cp('act', lg[0:Tn, :], psR[0:Tn, 0:36])
                T_ = slice(0, Tn)
                S.op('dve', lambda e, T_=T_, r1=r1, lg=lg: e.tensor_reduce(r1[T_, 0:1], lg[T_, 0:4], AX.X, ALU.max), [lg], [r1])
                S.ts('dve', gm[T_, :], lg[T_, 0:4], r1[T_, 0:1], None, ALU.is_equal)
                S.ts('dve', r2[T_, 0:4], lg[T_, 0:4], r1[T_, 0:1], None, ALU.subtract)
                S.memset('pool', r1[T_, 1:2], 0.0)
                S.act(r2[T_, 0:4], r2[T_, 0:4], AF.Exp, accum=r1[T_, 1:2])
                S.recip(r1[T_, 2:3], r1[T_, 1:2])
                S.ts('dve', r2[T_, 4:8], gm[T_, :], -1.0, 1e30, ALU.add, ALU.mult)
                S.tt('dve', els[T_, :].rearrange("p (g e) -> p g e", g=4), lg[T_, 4:36].rearrange("p (g e) -> p g e", g=4),
                     bc(r2[T_, 4:8], [Tn, 4, 8], 2), ALU.add)
                S.op('dve', lambda e, T_=T_, r1=r1, els=els: e.tensor_reduce(r1[T_, 3:4], els[T_, :], AX.X, ALU.max), [els], [r1])
                S.ts('dve', mk1[T_, :], els[T_, :], r1[T_, 3:4], None, ALU.is_equal)
                S.stt('dve', els[T_, :], mk1[T_, :], -1e30, els[T_, :], ALU.mult, ALU.add)
                S.op('dve', lambda e, T_=T_, r1=r1, els=els: e.tensor_reduce(r1[T_, 4:5], els[T_, :], AX.X, ALU.max), [els], [r1])
                S.ts('dve', mk2[T_, :], els[T_, :], r1[T_, 4:5], None, ALU.is_equal)
                S.tt('dve', r1[T_, 5:6], r1[T_, 4:5], r1[T_, 3:4], ALU.subtract)
                S.act(r1[T_, 5:6], r1[T_, 5:6], AF.Exp)
                S.ts('dve', r1[T_, 5:6], r1[T_, 5:6], 1.0, None, ALU.add)
                S.recip(r1[T_, 6:7], r1[T_, 5:6])
                S.tt('dve', r1[T_, 6:7], r1[T_, 6:7], r1[T_, 2:3], ALU.mult)
                S.tt('dve', r1[T_, 7:8], r1[T_, 2:3], r1[T_, 6:7], ALU.subtract)
                S.ts('dve', mk1[T_, :], mk1[T_, :], r1[T_, 6:7], None, ALU.mult)
                S.stt('dve', comb[T_, slot, :], mk2[T_, :], r1[T_, 7:8], mk1[T_, :], ALU.mult, ALU.add)
                S.cp('pool', actG[:, :, tok0:tok0 + Tn], xnT[:, 0:4, 0:Tn])
                S.cp('act', actR[:, :, tok0:tok0 + Tn], xnT[:, 4:8, 0:Tn])

            n1b = NSLOT if K_PH >= 1 else 0
            g1b = [slot1b(s_) for s_ in range(n1b)]
            if n1b:
                next(g1b[0])
            for s_ in range(n1b):
                if s_ + 1 < n1b:
                    next(g1b[s_ + 1])
                for _ in g1b[s_]:
                    pass
            Sched.keymap = {}
            S.barrier()
            S.emit()

        with ExitStack() as p3:
            sb = lambda n, s, d=F32: p3.enter_context(nc.sbuf_tensor("s_" + n, list(s), d))
            ps = lambda n, s, d=F32: p3.enter_context(nc.psum_tensor(n, list(s), d))
            psA = [ps("psA%d" % i, [128, 512]) for i in range(2)]
            psBm = [ps("psBm%d" % i, [128, 512]) for i in range(2)]
            psY = [ps("psY%d" % i, [128, 512]) for i in range(4)]
            w1b = [sb("w1b%d" % i, [128, 8, DE], BF16) for i in range(2)]
            w3b = [sb("w3b%d" % i, [128, 8, DE], BF16) for i in range(2)]
            w2b_ = [sb("w2mb%d" % i, [128, 4, D], BF16) for i in range(2)]
            stg = [sb("mstg%d" % i, [128, 1024]) for i in range(4)]
            hT = [sb("hT%d" % i, [128, 4, 512], BF16) for i in range(2)]
            s1 = [sb("s1_%d" % i, [128, 512]) for i in range(2)]
            si = [0]

            def load_expert(e):
                b = e % 2
                for q in range(4):
                    st = stg[si[0] % 4]; si[0] += 1
                    S.dma(st[:, :].rearrange("p (k c) -> p k c", k=2), w1_d[e, :, 2 * q:2 * q + 2, :])
                    S.cp('pool', w1b[b][:, 2 * q:2 * q + 2, :], st[:, :].rearrange("p (k c) -> p k c", k=2))
                for q in range(4):
                    st = stg[si[0] % 4]; si[0] += 1
                    S.dma(st[:, :].rearrange("p (k c) -> p k c", k=2), w3_d[e, :, 2 * q:2 * q + 2, :])
                    S.cp('pool', w3b[b][:, 2 * q:2 * q + 2, :], st[:, :].rearrange("p (k c) -> p k c", k=2))
                for q in range(4):
                    st = stg[si[0] % 4]; si[0] += 1
                    S.dma(st[:, :], w2m_d[e, :, q, :])
                    S.cp('pool', w2b_[b][:, q, :], st[:, :])
            blocks = [(b * 512, 512, [4 * b + j for j in range(4)]) for b in range(4)] + [(2048, 64, [16])]
            cnt = [0]
            pend = [None]
            nexp = min(NE, MOE_LIMIT) if K_PH >= 2 else 0
            if nexp > 0:
                load_expert(0)
            for e in range(nexp):
                if pend[0] is not None:
                    pend[0](); pend[0] = None
                if e + 1 < nexp:
                    load_expert(e + 1)
                b = e % 2
                for (t0, nt, slots) in blocks:
                    hb = hT[cnt[0] % 2]
                    for hc in range(4):
                        if hc == 1 and pend[0] is not None:
                            pend[0](); pend[0] = None
                        pa = psA[hc % 2]; pb_ = psBm[hc % 2]
                        for k in range(8):
                            a_ = actG if k < 4 else actR
                            S.mm(pa[:, 0:nt], w1b[b][:, k, hc * 128:(hc + 1) * 128], a_[:, k % 4, t0:t0 + nt], start=(k == 0), stop=(k == 7))
                        for k in range(8):
                            a_ = actG if k < 4 else actR
                            S.mm(pb_[:, 0:nt], w3b[b][:, k, hc * 128:(hc + 1) * 128], a_[:, k % 4, t0:t0 + nt], start=(k == 0), stop=(k == 7))
                        s_ = s1[hc % 2]
                        S.act(s_[:, 0:nt], pa[:, 0:nt], AF.Silu)
                        S.tt('dve', hb[:, hc, 0:nt], s_[:, 0:nt], pb_[:, 0:nt], ALU.mult)
                    def ymm(hb=hb, slots=slots, nt=nt, b=b, e=e):
                        for j, slot in enumerate(slots):
                            Tn = min(128, nt)
                            for hf in range(2):
                                py = psY[(2 * j + hf) % 4]
                                for hc in range(4):
                                    S.mm(py[0:Tn, :], hb[:, hc, j * 128:j * 128 + Tn], w2b_[b][:, hc, hf * 512:(hf + 1) * 512], start=(hc == 0), stop=(hc == 3))
                                xr = xres[0:Tn, slot, hf * 512:(hf + 1) * 512]
                                S.stt('dve', xr, py[0:Tn, :], comb[0:Tn, slot, e:e + 1], xr, ALU.mult, ALU.add)
                    pend[0] = ymm
                    cnt[0] += 1
            if pend[0] is not None:
                pend[0](); pend[0] = None
            S.barrier()
            S.emit()

        with ExitStack() as p4:
            sb = lambda n, s, d=F32: p4.enter_context(nc.sbuf_tensor("s_" + n, list(s), d))
            nfb = sb("nfb", [128, D]); junk3s = [sb("junk3_%d" % i, [128, D], BF16) for i in range(2)]
            ob = [sb("ob%d" % i, [128, D]) for i in range(2)]
            st3s = [sb("st3_%d" % i, [128, 4]) for i in range(2)]
            S.dma(nfb[:], nf_d.partition_broadcast(128))
            for slot in range(NSLOT if K_PH >= 3 else 0):
                Tn = 64 if slot == 16 else 128
                Sched.keymap = {'s_xres': ('xres', slot)}
                st3 = st3s[slot % 2]; junk3 = junk3s[slot % 2]
                S.memset('pool', st3[0:Tn, 0:1], 0.0)
                S.act(junk3[0:Tn, :], xres[0:Tn, slot, :], AF.Square, accum=st3[0:Tn, 0:1])
                S.act(st3[0:Tn, 1:2], st3[0:Tn, 0:1], AF.Ln, scale=1.0 / D, bias=1e-6)
                S.act(st3[0:Tn, 2:3], st3[0:Tn, 1:2], AF.Exp, scale=-0.5)
                o_ = ob[slot % 2]
                S.stt('dve' if slot % 2 == 0 else 'pool', o_[0:Tn, :], xres[0:Tn, slot, :], st3[0:Tn, 2:3], nfb[0:Tn, :], ALU.mult, ALU.mult)
                dst = ys_d if slot == 16 else yp_d[slot * 128:(slot + 1) * 128, :]
                S.dma(dst, o_[0:Tn, :])
            Sched.keymap = {}
            S.finish()
            print("sem counts", {str(k): v for k, v in S.count.items()})
            S.emit()
    return nc


def _consts():
    c = np.zeros((128, 10, 128), np.float32)
    i = np.arange(128)
    s, t = np.meshgrid(i, i, indexing='ij')
    c[:, 0] = (s == t)
    c[:, 1] = (s <= t)
    c[:, 2] = (s < t)
    c[:, 3] = (s > t)
    c[:, 4] = 1.0
    same = (s // 4 == t // 4)
    c[:, 5] = same & (s <= t)
    c[:, 6] = same & (s < t)
    c[:, 7] = same & (s > t)
    c[:, 8] = 1.0 / 64
    c[:, 9] = (s // 64 == t // 64)
    return c


_NC_CACHE = {}


def kernel(x_prompt, x_sample, state_gla, state_rwkv, state_shift, meta_tokens, norm_mix, w_in,
           gla_gate_w2, gla_gate_b, gla_norm, rwkv_mu, rwkv_w0, rwkv_w2, rwkv_a0, rwkv_a2, rwkv_g2,
           rwkv_kk, rwkv_ka, rwkv_rk, rwkv_ln_w, rwkv_ln_b, w_out, norm_ffn, router_group_w,
           router_group_b, router_expert_w, router_expert_b, moe_w1, moe_w3, moe_w2, norm_final):
    f = lambda a: np.ascontiguousarray(np.asarray(a, dtype=np.float32))
    x_prompt = f(x_prompt); x_sample = f(x_sample)
    win = f(np.asarray(w_in)[0].reshape(8, 128, IN_COLS).transpose(1, 0, 2))
    wout = f(np.asarray(w_out)[0].reshape(8, 128, D).transpose(1, 0, 2))
    col128 = lambda v: np.asarray(v, np.float32).reshape(-1, 128).T
    col64 = lambda v: np.asarray(v, np.float32).reshape(-1, 64).T
    mu = np.asarray(rwkv_mu)[0]
    v128 = f(np.concatenate([col128(norm_mix[0]), col128(norm_ffn[0]), col128(gla_norm[0]), col128(mu[1664:1792]),
                             col128(mu[0:1024]), col128(mu[1536:1664]), col128(rwkv_a0[0]), col128(rwkv_kk[0]),
                             col128(rwkv_ka[0]), col128(np.asarray(rwkv_rk)[0].reshape(-1))], axis=1))
    v64 = f(np.concatenate([col64(mu[0:1664]), col64(rwkv_w0[0]), col64(rwkv_a0[0]), col64(rwkv_kk[0]), col64(rwkv_ka[0]),
                            col64(np.asarray(rwkv_rk)[0].reshape(-1)), col64(rwkv_ln_w[0]), col64(rwkv_ln_b[0])], axis=1))
    wr_full = np.concatenate([np.asarray(router_group_w)[0], np.asarray(router_expert_w)[0].transpose(1, 0, 2).reshape(D, 32)], axis=1)
    wr = f(wr_full.reshape(8, 128, 36).transpose(1, 0, 2))
    rb = f(np.concatenate([np.asarray(router_group_b)[0], np.asarray(router_expert_b)[0].reshape(-1)])[None, :])
    w1 = f(np.asarray(moe_w1)[0].reshape(NE, 8, 128, DE).transpose(0, 2, 1, 3))
    w3 = f(np.asarray(moe_w3)[0].reshape(NE, 8, 128, DE).transpose(0, 2, 1, 3))
    w2m = f(np.asarray(moe_w2)[0].reshape(NE, 4, 128, D).transpose(0, 2, 1, 3))
    shared = dict(meta=f(meta_tokens), win=win, wout=wout, gw2=f(gla_gate_w2[0]), gb=f(gla_gate_b), w2=f(rwkv_w2[0]),
                  a2=f(rwkv_a2[0]), g2=f(rwkv_g2[0]), w0r=f(rwkv_w0), v128=v128, v64=v64, wr=wr, rb=rb,
                  nf=f(np.asarray(norm_final)[None, :]), w1=w1, w3=w3, w2m=w2m, cst=_consts())
    in_maps = []
    for c in range(NCORES):
        m = dict(shared)
        m["x"] = x_prompt[c]
        m["xs"] = f(x_sample[16 * c:16 * c + 16].reshape(64, D))
        m["sg"] = f(state_gla[0, 16 * c:16 * c + 16])
        m["sr"] = f(state_rwkv[0, 16 * c:16 * c + 16])
        m["ssh"] = f(state_shift[0, 16 * c:16 * c + 16])
        in_maps.append(m)
    if "nc" not in _NC_CACHE:
        _NC_CACHE["nc"] = build()
    res = run_bass_kernel_spmd(_NC_CACHE["nc"], in_maps, core_ids=list(range(NCORES)))
    R = res.results
    y_prompt = np.stack([R[c]["yp"] for c in range(NCORES)], 0).astype(np.float32)
    y_sample = np.concatenate([R[c]["ys"].reshape(16, 4, D) for c in range(NCORES)], 0).astype(np.float32)
    gla_p = np.stack([R[c]["glap"] for c in range(NCORES)], 0)[None].astype(np.float32)
    rwkv_p = np.stack([R[c]["rwp"] for c in range(NCORES)], 0)[None].astype(np.float32)
    shift_p = np.concatenate([R[c]["shp"] for c in range(NCORES)], 0)[None].astype(np.float32)
    gla_s = np.concatenate([R[c]["glas"] for c in range(NCORES)], 0)[None].astype(np.float32)
    rwkv_s = np.concatenate([R[c]["rws"] for c in range(NCORES)], 0)[None].astype(np.float32)
    shift_s = np.concatenate([R[c]["shs"] for c in range(NCORES)], 0)[None].astype(np.float32)
    return (y_prompt, y_sample, gla_p, rwkv_p, shift_p, gla_s, rwkv_s, shift_s)
```

```python
import os
import numpy as np
from contextlib import ExitStack
import concourse.bass as bass
import concourse.mybir as mybir
from concourse.bass_utils import run_bass_kernel_spmd

F32 = mybir.dt.float32
BF16 = mybir.dt.bfloat16
ALU = mybir.AluOpType
AF = mybir.ActivationFunctionType
AX = mybir.AxisListType

NDMA = 24
NCORES = 8
D = 1024
GLA_COLS = 1552
RW_COLS = 1792
IN_COLS = 3344
NE = 32
DE = 512
NPT = 16
NSLOT = 17
NTOK = 2048 + 64
DECAY = -0.606531
MOE_LIMIT = int(os.environ.get("K_MOE_LIMIT", "32"))
K_NT = int(os.environ.get("K_NT", "18"))
K_STOP = int(os.environ.get("K_STOP", "99"))


class _Stop(Exception):
    pass


def _chk(n):
    if K_STOP <= n:
        raise _Stop()


K_PH = int(os.environ.get("K_PH", "9"))


class Sched:
    def __init__(self, nc, es):
        self.nc = nc
        self.engs = ['pe', 'act', 'dve', 'pool', 'sp']
        self.ops = {e: [] for e in self.engs}
        self.count = {}
        self.seen = {e: {} for e in self.engs}
        self.last_w = {}
        self.readers = {}
        self.sems = {}
        for k in ['pe', 'act', 'dve', 'pool']:
            self.sems[k] = es.enter_context(nc.semaphore('s_' + k))
            self.count[k] = 0
        for i in range(NDMA):
            k = ('dma', i)
            self.sems[k] = es.enter_context(nc.semaphore('s_dma%d' % i))
            self.count[k] = 0
        self.dma_rr = 0

    keymap = {}

    @staticmethod
    def key(x):
        if isinstance(x, (str, tuple)):
            return x
        n = x.tensor.name if hasattr(x, 'tensor') else x.name
        return Sched.keymap.get(n, n)

    def op(self, eng, fn, reads, writes, dma=False):
        reads = [self.key(r) for r in reads]
        writes = [self.key(w) for w in writes]
        deps = []
        for b in reads:
            if b in self.last_w:
                deps.append(self.last_w[b])
            if isinstance(b, str) and b.startswith('ps'):
                deps.extend(ev for ev in self.readers.get(b, []) if ev[0] != eng)
        for b in writes:
            if b in self.last_w:
                deps.append(self.last_w[b])
            deps.extend(self.readers.get(b, []))
        seen = self.seen[eng]
        wd = {}
        for (k, v) in deps:
            if k == 'pe' and eng == 'pe':
                continue
            if seen.get(k, 0) >= v:
                continue
            seen[k] = v
            wd[k] = max(wd.get(k, 0), v)
        if dma:
            k = ('dma', self.dma_rr % NDMA)
            self.dma_rr += 1
            if self.count[k] > 0 and seen.get(k, 0) < self.count[k]:
                seen[k] = self.count[k]
                wd[k] = self.count[k]
            self.count[k] += 16
            ev = (k, self.count[k])
            inc = (k, 16)
        else:
            self.count[eng] += 1
            ev = (eng, self.count[eng])
            inc = (eng, 1)
        self.ops[eng].append((fn, list(wd.items()), inc))
        for b in writes:
            self.last_w[b] = ev
            self.readers[b] = []
        for b in reads:
            if b not in writes:
                self.readers.setdefault(b, []).append(ev)
        return ev

    def barrier(self):
        snap = dict(self.count)
        for e in self.engs:
            waits = []
            for k, v in snap.items():
                if v == 0 or k == e:
                    continue
                if self.seen[e].get(k, 0) >= v:
                    continue
                self.seen[e][k] = v
                waits.append((k, v))
            if waits:
                self.ops[e].append((None, waits, None))

    def finish(self):
        waits = [(k, v) for k, v in self.count.items() if v > 0]
        self.ops['sp'].append((None, waits, None))

    def emit(self):
        nc = self.nc
        engobj = {'pe': 'tensor', 'act': 'scalar', 'dve': 'vector', 'pool': 'gpsimd', 'sp': 'sync'}
        sems = self.sems
        with nc.allow_low_precision("bf16 matmul operands by design"), nc.Block() as block:
            for e in self.engs:
                def body(eng, ops=self.ops[e]):
                    for fn, waits, inc in ops:
                        for k, v in waits:
                            eng.wait_ge(sems[k], v)
                        if fn is None:
                            continue
                        ins = fn(eng)
                        ins.then_inc(sems[inc[0]], inc[1])
                getattr(block, engobj[e])(body)
        self.ops = {e: [] for e in self.engs}

    def mm(self, out, lhsT, rhs, start=True, stop=True):
        r = [lhsT, rhs] + ([] if start else [out])
        return self.op('pe', lambda e: e.matmul(out, lhsT, rhs, start=start, stop=stop), r, [out])

    def tr(self, out, in_, ident):
        return self.op('pe', lambda e: e.transpose(out, in_, ident), [in_, ident], [out])

    def act(self, out, in_, func=None, bias=0.0, scale=1.0, accum=None):
        func = AF.Copy if func is None else func
        r = [in_] + [x for x in (bias, scale) if not isinstance(x, (int, float))]
        w = [out] + ([accum] if accum is not None else [])
        kw = {}
        if accum is not None:
            kw['accum_out'] = accum
        return self.op('act', lambda e: e.activation(out, in_, func, bias=bias, scale=scale, **kw), r, w)

    def tt(self, eng, out, a, b, op):
        return self.op(eng, lambda e: e.tensor_tensor(out, a, b, op), [a, b], [out])

    def ts(self, eng, out, a, s1, s2=None, op0=ALU.mult, op1=None):
        r = [a] + [x for x in (s1, s2) if x is not None and not isinstance(x, (int, float))]
        if op1 is None:
            return self.op(eng, lambda e: e.tensor_scalar(out, a, s1, s2, op0), r, [out])
        return self.op(eng, lambda e: e.tensor_scalar(out, a, s1, s2, op0, op1), r, [out])

    def stt(self, eng, out, a, s, b, op0, op1):
        r = [a, b] + ([] if isinstance(s, (int, float)) else [s])
        eng = 'dve'
        return self.op(eng, lambda e: e.scalar_tensor_tensor(out, a, s, b, op0, op1), r, [out])

    def cp(self, eng, out, a):
        if eng == 'act':
            return self.act(out, a)
        return self.op(eng, lambda e: e.tensor_copy(out, a), [a], [out])

    def recip(self, out, a):
        return self.op('dve', lambda e: e.reciprocal(out, a), [a], [out])

    def memset(self, eng, out, val):
        return self.op(eng, lambda e: e.memset(out, val), [], [out])

    def dma(self, out, in_, eng='sp', **kw):
        return self.op(eng, lambda e: e.dma_start(out=out, in_=in_, **kw), [in_], [out], dma=True)


def bc(ap, shape, axis):
    return ap.unsqueeze(axis).to_broadcast(list(shape))


def build():
    nc = bass.Bass("TRN2", target_bir_lowering=False)
    din = lambda n, s: nc.dram_tensor(n, list(s), F32, kind="ExternalInput").ap()
    dout = lambda n, s: nc.dram_tensor(n, list(s), F32, kind="ExternalOutput").ap()
    x_d = din("x", [2048, D]); xs_d = din("xs", [64, D]); meta_d = din("meta", [16, D])
    sg_d = din("sg", [16, 4, 64, 128]); sr_d = din("sr", [16, 8, 64, 64]); ssh_d = din("ssh", [16, RW_COLS])
    win_d = din("win", [128, 8, IN_COLS])
    wout_d = din("wout", [128, 8, D])
    gw2_d = din("gw2", [16, 256]); gb_d = din("gb", [1, 256])
    w2_d = din("w2", [64, 512]); a2_d = din("a2", [64, 512]); g2_d = din("g2", [128, 512])
    w0r_d = din("w0r", [1, 512])
    v128_d = din("v128", [128, 43]); v64_d = din("v64", [64, 82])
    wr_d = din("wr", [128, 8, 36]); rb_d = din("rb", [1, 36])
    nf_d = din("nf", [1, D])
    w1_d = din("w1", [NE, 128, 8, DE]); w3_d = din("w3", [NE, 128, 8, DE]); w2m_d = din("w2m", [NE, 128, 4, D])
    cst_d = din("cst", [128, 10, 128])
    yp_d = dout("yp", [2048, D]); ys_d = dout("ys", [64, D])
    glap_d = dout("glap", [4, 64, 128]); rwp_d = dout("rwp", [8, 64, 64]); shp_d = dout("shp", [1, RW_COLS])
    glas_d = dout("glas", [16, 4, 64, 128]); rws_d = dout("rws", [16, 8, 64, 64]); shs_d = dout("shs", [16, RW_COLS])

    with ExitStack() as es:
        S = Sched(nc, es)
        sbG = lambda n, s, d=F32: es.enter_context(nc.sbuf_tensor("s_" + n, list(s), d))
        cst = sbG("cst", [128, 10, 128])
        identf = cst[:, 0, :]; triu_i = cst[:, 1, :]; triu_s = cst[:, 2, :]; tril_s = cst[:, 3, :]; onesf = cst[:, 4, :]
        blk_i = cst[:, 5, :]; blk_us = cst[:, 6, :]; blk_ls = cst[:, 7, :]; ones64 = cst[:, 8, :]
        identb = sbG("identb", [128, 128], BF16)
        actG = sbG("actG", [128, 4, NTOK], BF16)
        actR = sbG("actR", [128, 4, NTOK], BF16)
        v128 = sbG("v128", [128, 43]); v64 = sbG("v64", [64, 82])
        S.dma(cst[:], cst_d); S.dma(v128[:], v128_d); S.dma(v64[:], v64_d)
        S.cp('dve', identb[:], identf)
        g_mix = v128[:, 0:8]; g_ffn = v128[:, 8:16]; gnorm = v128[:, 16:17]; mu_g = v128[:, 17:18]
        mu128 = v128[:, 18:27]; a0c128 = v128[:, 27:31]; kkw128 = v128[:, 31:35]; kac128 = v128[:, 35:39]; rkc128 = v128[:, 39:43]
        blkones = cst[:, 9, :]
        mu64 = v64[:, 0:26]; a0c = v64[:, 34:42]; kkw = v64[:, 42:50]; kac = v64[:, 50:58]
        rkc = v64[:, 58:66]; lnw = v64[:, 66:74]; lnb = v64[:, 74:82]

        winb_d = nc.dram_tensor("winb_scr", [128, 8, IN_COLS], BF16).ap()
        with ExitStack() as p1:
            sb = lambda n, s, d=F32: p1.enter_context(nc.sbuf_tensor("s_" + n, list(s), d))
            ps = lambda n, s, d=F32: p1.enter_context(nc.psum_tensor(n, list(s), d))
            psF = [ps("psF%d" % i, [128, 512]) for i in range(4)]
            psB = [ps("psB%d" % i, [128, 1024], BF16) for i in range(2)]
            psW = ps("psW", [128, 1024])
            rr = {'a': 0, 'b': 0}

            def pfA():
                rr['a'] += 1
                return psF[rr['a'] % 2]

            def pfB():
                rr['b'] += 1
                return psF[2 + rr['b'] % 2]

            sc = [sb("sc%d" % i, [128, 512]) for i in range(8)]
            s2 = [sb("s2_%d" % i, [128, 512]) for i in range(5)]
            v64v = lambda t: t[0:64, :].rearrange("p (h t) -> p h t", h=4)

            cb16 = [sb("cb16_%d" % i, [128, 512], BF16) for i in range(4)]
            pieces = [(k, c0, min(512, IN_COLS - c0)) for k in range(8) for c0 in range(0, IN_COLS, 512)]
            stin = sc[0:6]
            for i in range(min(6, len(pieces))):
                k, c0, w_ = pieces[i]
                S.dma(stin[i % 6][:, 0:w_], win_d[:, k, c0:c0 + w_])
            for i, (k, c0, w_) in enumerate(pieces):
                S.cp(['dve', 'pool'][i % 2], cb16[i % 4][:, 0:w_], stin[i % 6][:, 0:w_])
                S.op('act', lambda e, o=winb_d[:, k, c0:c0 + w_], a=cb16[i % 4][:, 0:w_]: e.dma_start(out=o, in_=a),
                     [cb16[i % 4]], [('winb_piece', i)], dma=True)
                if i + 6 < len(pieces):
                    k2, c2, w2_ = pieces[i + 6]
                    S.dma(stin[(i + 6) % 6][:, 0:w2_], win_d[:, k2, c2:c2 + w2_])
            S.barrier()
            wbuf = [sb("wbuf%d" % i, [128, 8, 256], BF16) for i in range(3)]
            wi = [0]
            gw2f = sb("gw2f", [16, 256]); gbf = sb("gbf", [1, 256])
            w2b = sb("w2b", [64, 512], BF16); a2b128 = sb("a2b128", [128, 512], BF16); g2b = sb("g2b", [128, 512], BF16)
            w0r = sb("w0r", [1, 512])
            S.dma(gw2f[:], gw2_d); S.dma(gbf[:], gb_d)
            S.dma(sc[4][0:64, :], w2_d); S.cp('pool', w2b[:], sc[4][0:64, :])
            S.dma(sc[5][64:128, :], a2_d); S.cp('pool', a2b128[64:128, :], sc[5][64:128, :])
            S.dma(sc[6][:, :], g2_d); S.cp('pool', g2b[:], sc[6][:, :])
            S.dma(w0r[:], w0r_d)
            omu64 = sb("omu64", [64, 26]); omu_g = sb("omu_g", [128, 1]); omu128 = sb("omu128", [128, 9])
            S.ts('dve', omu128[:], mu128, -1.0, 1.0, ALU.mult, ALU.add)
            v128v = lambda t: t[:, :].rearrange("p (h t) -> p h t", h=4)
            S.ts('dve', omu64[:], mu64, -1.0, 1.0, ALU.mult, ALU.add)
            S.ts('dve', omu_g[:], mu_g, -1.0, 1.0, ALU.mult, ALU.add)
            m4 = sb("m4", [128, 4, 128])
            for q in range(4):
                S.cp('pool', m4[:, q, :], triu_s if q % 2 == 0 else triu_i)
            xt = sb("xt", [128, D]); xsb = sb("xsb", [128, D], BF16)
            stat1 = sb("stat1", [128, 4])
            nT = sb("nT", [128, 8, 128], BF16)
            carry = [sb("carry%d" % i, [64, 26]) for i in range(2)]
            carryG = [sb("carryG%d" % i, [128, 1]) for i in range(2)]
            carry128 = [sb("carry128_%d" % i, [128, 9]) for i in range(2)]
            shT128 = sb("shT128", [128, 9, 16])
            shT = sb("shT", [64, 26, 16]); shTG = sb("shTG", [128, 16])
            xr = sb("xr", [128, 4, 128]); xk = sb("xk", [128, 4, 128])
            shrow = sb("shrow", [64, RW_COLS])
            xwa = sb("xwa", [128, 1, 128]); xmG = sb("xmG", [128, 128]); t4g = sb("t4g", [128, 128])
            glT = sb("glT", [16, 128])
            twl = sb("twl", [64, 128]); twlb = sb("twlb", [64, 128], BF16); alb128 = sb("alb128", [128, 128], BF16)
            blkonesb = sb("blkonesb", [128, 128], BF16)
            S.cp('pool', blkonesb[:], blkones)
            sgl = sb("sgl", [128, 128]); slw = sb("slw", [128, 512])
            HB = []
            for i in range(2):
                n_ = lambda s_: "%s_%d" % (s_, i)
                HB.append(dict(
                    vT=sb(n_("vT"), [128, 4, 128], BF16), ggT=sb(n_("ggT"), [128, 4, 128]),
                    qs=sb(n_("qs"), [64, 4, 128], BF16), ks=sb(n_("ks"), [64, 4, 128], BF16), khT=sb(n_("khT"), [64, 4, 128], BF16),
                    gam=sb(n_("gam"), [64, 4, 16]), WCs=sb(n_("WCs"), [64, 8, 16]), vTb=sb(n_("vTb"), [64, 8, 128], BF16),
                    ARt=sb(n_("ARt"), [128, 4, 2, 128], BF16), BtT=sb(n_("BtT"), [128, 4, 128], BF16), KtT=sb(n_("KtT"), [128, 4, 128], BF16),
                    rkr=sb(n_("rkr"), [128, 4, 128], BF16), sglb=sb(n_("sglb"), [128, 128], BF16)))
            Vg = sb("Vg", [128, 768], BF16); scT = sb("scT", [128, 4, 128], BF16)
            ogT = sb("ogT", [128, 4, 128]); Sg = sb("Sg", [64, 4, 128]); Sgb = sb("Sgb", [64, 4, 128], BF16)
            psB1f = psB[1][:, :].bitcast(F32)
            rhsPQ = sb("rhsPQ", [128, 8, 2, 64], BF16); tokB = sb("tokB", [128, 8, 64], BF16)
            tokKV = sb("tokKV", [128, 2, 8, 64], BF16)
            Mh = [sb("Mh%d" % h, [128, 4, 128], BF16) for h in range(8)]
            MhS = sb("MhS", [16, 8, 4, 16], BF16)
            YZ = [sb("YZ%d" % i, [128, 4, 2, 128], BF16) for i in range(2)]
            TT = [sb("TT%d" % i, [128, 4, 128], BF16) for i in range(2)]
            PQ = sb("PQ", [128, 8, 2, 64], BF16)
            GT = sb("GT", [64, 8, 64]); HW = sb("HW", [64, 8, 64]); ATb = sb("ATb", [64, 8, 128], BF16)
            Sr = sb("Sr", [64, 8, 64]); Srb = sb("Srb", [64, 8, 64], BF16)
            yT = sb("yT", [64, 8, 128]); oRo = sb("oRo", [64, 4, 128], BF16)
            onesb = sb("onesb", [64, 64], BF16)
            S.cp('pool', onesb[:], onesf[0:64, 0:64])

            for cb in carry:
                S.memset('pool', cb[:], 0.0)
            for cb in carryG:
                S.memset('pool', cb[:], 0.0)
            for cb in carry128:
                S.memset('pool', cb[:], 0.0)
            S.memset('dve', Sg[:], 0.0); S.memset('dve', Sr[:], 0.0)
            S.memset('pool', Sgb[:], 0.0); S.memset('pool', Srb[:], 0.0)
            S.dma(shrow[0:16, :], ssh_d)
            for g in range(26):
                S.tr(psW[0:64, g * 16:(g + 1) * 16], shrow[0:16, g * 64:(g + 1) * 64], identf[0:16, 0:16])
            S.cp('act', shT[:], psW[0:64, 0:416].rearrange("p (g s) -> p g s", g=26))
            p_ = pfA()
            for gi, c0 in enumerate([0, 128, 256, 384, 512, 640, 768, 896, 1536]):
                S.tr(p_[:, gi * 16:(gi + 1) * 16], shrow[0:16, c0:c0 + 128], identf[0:16, 0:16])
            S.cp('act', shT128[:], p_[:, 0:144].rearrange("p (g s) -> p g s", g=9))
            p_ = pfA()
            S.tr(p_[:, 0:16], shrow[0:16, 1664:1792], identf[0:16, 0:16])
            S.cp('act', shTG[:], p_[:, 0:16])
            tcount = [0]

            def tile_info(slot):
                sample = (slot == 16)
                Tn = 16 if slot < 0 else (64 if sample else 128)
                tok0 = 2048 if sample else slot * 128
                return sample, Tn, tok0

            def st1(slot, H):
                sample, Tn, tok0 = tile_info(slot)
                if slot < 0:
                    src = meta_d
                elif sample:
                    src = xs_d
                else:
                    src = x_d[slot * 128:(slot + 1) * 128, :]
                mi = blk_i if sample else triu_i
                mus = blk_us if sample else triu_s
                mls = blk_ls if sample else tril_s
                c_old = carry[tcount[0] % 2]; c_new = carry[(tcount[0] + 1) % 2]
                cg_old = carryG[tcount[0] % 2]; cg_new = carryG[(tcount[0] + 1) % 2]
                c128_old = carry128[tcount[0] % 2]; c128_new = carry128[(tcount[0] + 1) % 2]
                tcount[0] += 1
                vT = H['vT']; ggT = H['ggT']; qs = H['qs']; ks = H['ks']; khT = H['khT']; gam = H['gam']; WCs = H['WCs']
                vTb = H['vTb']; ARt = H['ARt']; BtT = H['BtT']; KtT = H['KtT']; rkr = H['rkr']; sglb = H['sglb']
                S.dma(xt[0:Tn, :], src)
                S.memset('pool', stat1[0:Tn, 0:1], 0.0)
                S.act(xsb[0:Tn, :], xt[0:Tn, :], AF.Square, accum=stat1[0:Tn, 0:1])
                S.act(stat1[0:Tn, 1:2], stat1[0:Tn, 0:1], AF.Ln, scale=1.0 / D, bias=1e-6)
                S.act(stat1[0:Tn, 2:3], stat1[0:Tn, 1:2], AF.Exp, scale=-0.5)
                S.ts('dve', xsb[0:Tn, :], xt[0:Tn, :], stat1[0:Tn, 2:3], None, ALU.mult)
                p_ = psB[0]
                for k in range(8):
                    S.tr(p_[:, k * 128:k * 128 + Tn], xsb[0:Tn, k * 128:(k + 1) * 128], identb[0:Tn, 0:Tn])
                S.tt('dve', nT[:, :, 0:Tn], p_[:, :].rearrange("p (k t) -> p k t", k=8)[:, :, 0:Tn],
                     bc(g_mix, [128, 8, Tn], 2), ALU.mult)
                yield

                def proj(c0, M, ngroups, dst_fn):
                    per = 256 // M if M >= 64 else 1
                    g = 0
                    while g < ngroups:
                        n = min(per, ngroups - g)
                        wb = wbuf[wi[0] % 3]; wi[0] += 1
                        S.dma(wb[:, :, 0:n * M], winb_d[:, :, c0 + g * M:c0 + (g + n) * M])
                        p_ = pfA()
                        for j in range(n):
                            for k in range(8):
                                S.mm(p_[0:M, j * 128:j * 128 + Tn], wb[:, k, j * M:(j + 1) * M], nT[:, k, 0:Tn],
                                     start=(k == 0), stop=(k == 7))
                        dst_fn(g, n, p_[0:M, 0:n * 128].rearrange("p (j t) -> p j t", j=n))
                        g += n
                qTv = v64v(sc[0]); kTv = v64v(sc[1])
                proj(0, 64, 4, lambda g, n, v: S.cp('act', qTv[:, g:g + n, 0:Tn], v[:, :, 0:Tn]))
                proj(256, 64, 4, lambda g, n, v: S.cp('act', kTv[:, g:g + n, 0:Tn], v[:, :, 0:Tn]))
                yield
                proj(512, 128, 4, lambda g, n, v: S.cp('act', vT[:, g:g + n, 0:Tn], v[:, :, 0:Tn]))
                proj(1024, 128, 4, lambda g, n, v: S.cp('dve', ggT[:, g:g + n, 0:Tn], v[:, :, 0:Tn]))
                proj(1536, 16, 1, lambda g, n, v: S.cp('act', glT[:, 0:Tn], v[:, 0, 0:Tn]))
                yield
                p_ = pfA()
                S.mm(p_[0:Tn, 0:256], glT[0:16, 0:Tn], gw2f[:, :], start=True, stop=False)
                S.mm(p_[0:Tn, 0:256], onesf[0:1, 0:Tn], gbf[0:1, :], start=False, stop=True)
                S.act(sc[2][0:Tn, 0:256], p_[0:Tn, 0:256], AF.Exp, scale=-1.0)
                S.act(sc[2][0:Tn, 256:512], sc[2][0:Tn, 0:256], AF.Ln, bias=1.0)
                lgt = sc[2][:, 256:512]
                EQ = v64v(sc[3]); EK = v64v(sc[4]); ED = v64v(sc[5])
                for hp in range(2):
                    p_ = pfA()
                    for hh in range(2):
                        h = hp * 2 + hh
                        S.mm(p_[0:64, hh * 256:hh * 256 + Tn], lgt[0:Tn, h * 64:(h + 1) * 64], mi[0:Tn, 0:Tn])
                        S.mm(p_[0:64, hh * 256 + 128:hh * 256 + 128 + Tn], lgt[0:Tn, h * 64:(h + 1) * 64], mls[0:Tn, 0:Tn])
                    cs = p_[0:64, :].rearrange("p (h a t) -> p h a t", h=2, a=2)
                    hs = slice(hp * 2, hp * 2 + 2)
                    S.act(EQ[:, hs, 0:Tn], cs[:, :, 0, 0:Tn], AF.Exp, scale=-1.0 / 16)
                    S.act(EK[:, hs, 0:Tn], cs[:, :, 0, 0:Tn], AF.Exp, scale=1.0 / 16)
                    S.act(ED[:, hs, 0:Tn], cs[:, :, 1, 0:Tn], AF.Exp, scale=-1.0 / 16)
                S.stt('dve', qs[:, :, 0:Tn], qTv[:, :, 0:Tn], 0.125, EQ[:, :, 0:Tn], ALU.mult, ALU.mult)
                S.tt('pool', ks[:, :, 0:Tn], kTv[:, :, 0:Tn], EK[:, :, 0:Tn], ALU.mult)
                S.tt('dve', khT[:, :, 0:Tn], kTv[:, :, 0:Tn], ED[:, :, 0:Tn], ALU.mult)
                if sample:
                    S.cp('pool', gam[:, :, :], EQ[:, :, 0:64].rearrange("p h (s t) -> p h s t", t=4)[:, :, :, 3])
                else:
                    S.cp('pool', gam[:, :, 0:1], EQ[:, :, Tn - 1:Tn])
                yield

                bi = [0]

                def rw_evac128(g0, n, v, dstf):
                    t4 = v128v(sc[6 + bi[0] % 2]); bi[0] += 1
                    dst = dstf[:, :, 0:Tn]
                    cur = v[:, :, 0:Tn]
                    mu_ = mu128[:, g0:g0 + n]; omu_ = omu128[:, g0:g0 + n]
                    S.tt('dve', dst, cur, bc(omu_, [128, n, Tn], 2), ALU.mult)
                    if sample:
                        c4 = cur.rearrange("p g (s t) -> p g s t", t=4)
                        t44 = t4[:, 0:n, 0:Tn].rearrange("p g (s t) -> p g s t", t=4)
                        S.tt('dve', t44[:, :, :, 1:4], c4[:, :, :, 0:3],
                             mu_.unsqueeze(2).unsqueeze(3).to_broadcast([128, n, 16, 3]), ALU.mult)
                        S.tt('pool', t44[:, :, :, 0], shT128[:, g0:g0 + n, :], bc(mu_, [128, n, 16], 2), ALU.mult)
                    else:
                        S.tt('dve', t4[:, 0:n, 1:Tn], v[:, :, 0:Tn - 1], bc(mu_, [128, n, Tn - 1], 2), ALU.mult)
                        S.tt('pool', t4[:, 0:n, 0:1], c128_old[:, g0:g0 + n].unsqueeze(2), mu_.unsqueeze(2), ALU.mult)
                        S.cp('dve', c128_new[:, g0:g0 + n].unsqueeze(2), v[:, :, Tn - 1:Tn])
                    S.tt('pool', dst, dst, t4[:, 0:n, 0:Tn], ALU.add)

                def rw_evac(g0, n, v):
                    t4 = v64v(sc[6 + bi[0] % 2]); bi[0] += 1
                    dst = vTb[:, g0 - 16:g0 - 16 + n, 0:Tn]
                    cur = v[:, :, 0:Tn]
                    mu_ = mu64[:, g0:g0 + n]; omu_ = omu64[:, g0:g0 + n]
                    S.tt('dve', dst, cur, bc(omu_, [64, n, Tn], 2), ALU.mult)
                    if sample:
                        c4 = cur.rearrange("p g (s t) -> p g s t", t=4)
                        t44 = t4[:, 0:n, 0:Tn].rearrange("p g (s t) -> p g s t", t=4)
                        S.tt('dve', t44[:, :, :, 1:4], c4[:, :, :, 0:3],
                             mu_.unsqueeze(2).unsqueeze(3).to_broadcast([64, n, 16, 3]), ALU.mult)
                        S.tt('pool', t44[:, :, :, 0], shT[:, g0:g0 + n, :], bc(mu_, [64, n, 16], 2), ALU.mult)
                    else:
                        S.tt('dve', t4[:, 0:n, 1:Tn], v[:, :, 0:Tn - 1], bc(mu_, [64, n, Tn - 1], 2), ALU.mult)
                        S.tt('pool', t4[:, 0:n, 0:1], c_old[:, g0:g0 + n].unsqueeze(2), mu_.unsqueeze(2), ALU.mult)
                        S.cp('dve', c_new[:, g0:g0 + n].unsqueeze(2), v[:, :, Tn - 1:Tn])
                    S.tt('pool', dst, dst, t4[:, 0:n, 0:Tn], ALU.add)
                proj(GLA_COLS, 128, 4, lambda g, n, v: rw_evac128(g, n, v, xr[:, g:g + n, :]))
                yield
                proj(GLA_COLS + 512, 128, 4, lambda g, n, v: rw_evac128(4 + g, n, v, xk[:, g:g + n, :]))
                yield
                proj(GLA_COLS + 1024, 64, 8, lambda g, n, v: rw_evac(g + 16, n, v))
                proj(GLA_COLS + 1536, 128, 1, lambda g, n, v: rw_evac128(8, 1, v, xwa[:, 0:1, :]))
                yield

                def rwg_evac(g0, n, v):
                    cur = v[:, 0, 0:Tn]
                    S.ts('dve', xmG[:, 0:Tn], cur, omu_g[:, 0:1], None, ALU.mult)
                    if sample:
                        c3 = cur.rearrange("p (s t) -> p s t", t=4)
                        t3 = t4g[:, 0:Tn].rearrange("p (s t) -> p s t", t=4)
                        S.ts('dve', t3[:, :, 1:4], c3[:, :, 0:3], mu_g, None, ALU.mult)
                        S.ts('pool', t3[:, :, 0], shTG[:, :], mu_g, None, ALU.mult)
                    else:
                        S.ts('dve', t4g[:, 1:Tn], v[:, 0, 0:Tn - 1], mu_g, None, ALU.mult)
                        S.ts('pool', t4g[:, 0:1], cg_old[:, 0:1], mu_g, None, ALU.mult)
                        S.cp('dve', cg_new[:, 0:1], v[:, 0, Tn - 1:Tn])
                    S.tt('pool', xmG[:, 0:Tn], xmG[:, 0:Tn], t4g[:, 0:Tn], ALU.add)
                proj(GLA_COLS + 1664, 128, 1, rwg_evac)
                rT = xr[:, :, 0:Tn]; kraw = xk[:, :, 0:Tn]
                S.act(twl[:, 0:Tn], xwa[0:64, 0, 0:Tn], AF.Exp, scale=-2.0)
                S.act(twl[:, 0:Tn], twl[:, 0:Tn], AF.Ln, bias=1.0)
                S.act(twl[:, 0:Tn], twl[:, 0:Tn], AF.Exp, scale=-1.0)
                S.ts('dve', twlb[:, 0:Tn], twl[:, 0:Tn], 2.0, -1.0, ALU.mult, ALU.add)
                S.cp('pool', alb128[64:128, 0:Tn], xwa[64:128, 0, 0:Tn])
                S.act(sgl[:, 0:Tn], xmG[:, 0:Tn], AF.Exp, scale=-1.0)
                S.act(sgl[:, 0:Tn], sgl[:, 0:Tn], AF.Ln, bias=1.0)
                S.act(sglb[:, 0:Tn], sgl[:, 0:Tn], AF.Exp, scale=-1.0)
                p_ = pfA()
                S.mm(p_[0:Tn, :], twlb[:, 0:Tn], w2b[:, :], start=True, stop=False)
                S.mm(p_[0:Tn, :], onesf[0:1, 0:Tn], w0r[0:1, :], start=False, stop=True)
                S.act(slw[0:Tn, :], p_[0:Tn, :], AF.Exp, scale=-1.0)
                S.act(slw[0:Tn, :], slw[0:Tn, :], AF.Ln, bias=1.0)
                S.act(slw[0:Tn, :], slw[0:Tn, :], AF.Exp, scale=-1.0)
                yield
                ER = v128v(sc[0])[:, :, 0:Tn]; EB = v128v(sc[1])[:, :, 0:Tn]; EA = v128v(sc[2])[:, :, 0:Tn]
                aT = v128v(sc[3])[:, :, 0:Tn]; kk = v128v(sc[4])[:, :, 0:Tn]; s5 = v128v(sc[5])[:, :, 0:Tn]
                kp = v128v(sc[6])[:, :, 0:Tn]
                for hp in range(2):
                    p_ = pfA()
                    for cc in range(2):
                        c = hp * 2 + cc
                        S.mm(p_[:, cc * 256:cc * 256 + Tn], slw[0:Tn, c * 128:(c + 1) * 128], mi[0:Tn, 0:Tn])
                        S.mm(p_[:, cc * 256 + 128:cc * 256 + 128 + Tn], slw[0:Tn, c * 128:(c + 1) * 128], mus[0:Tn, 0:Tn])
                    cw = p_[:, :].rearrange("p (h a t) -> p h a t", h=2, a=2)
                    h2 = slice(hp * 2, hp * 2 + 2)
                    S.act(v128v(sc[0])[:, h2, 0:Tn], cw[:, :, 0, 0:Tn], AF.Exp, scale=DECAY)
                    S.act(v128v(sc[1])[:, h2, 0:Tn], cw[:, :, 0, 0:Tn], AF.Exp, scale=-DECAY)
                    S.act(v128v(sc[2])[:, h2, 0:Tn], cw[:, :, 1, 0:Tn], AF.Exp, scale=DECAY)
                p_ = pfA()
                ncol = 16 if sample else 1
                selr = (blk_i[0:64, 0:64].rearrange("p (s t) -> p s t", t=4)[:, :, 3]) if sample else onesf[0:Tn, 0:1]
                for h in range(8):
                    S.mm(p_[0:64, h * 16:h * 16 + ncol], slw[0:Tn, h * 64:(h + 1) * 64], selr)
                S.act(WCs[:, :, 0:ncol], p_[0:64, 0:128].rearrange("p (h s) -> p h s", h=8)[:, :, 0:ncol], AF.Exp, scale=DECAY)
                p_ = pfA()
                for c in range(4):
                    S.mm(p_[:, c * 128:c * 128 + Tn], a2b128[64:128, c * 128:(c + 1) * 128], alb128[64:128, 0:Tn])
                pa = p_[:, :].rearrange("p (h t) -> p h t", h=4)[:, :, 0:Tn]
                S.tt('dve', aT, pa, bc(a0c128, [128, 4, Tn], 2), ALU.add)
                S.act(aT, aT, AF.Exp, scale=-1.0)
                S.act(aT, aT, AF.Ln, bias=1.0)
                S.act(aT, aT, AF.Exp, scale=-1.0)
                yield
                S.tt('pool', kk, kraw, bc(kkw128, [128, 4, Tn], 2), ALU.mult)
                S.tt('pool', s5, kk, kk, ALU.mult)
                p_ = pfA()
                for c in range(4):
                    S.mm(p_[:, c * 128:c * 128 + Tn], blkones[:, :], v128v(sc[5])[:, c, 0:Tn])
                pk = p_[:, :].rearrange("p (h t) -> p h t", h=4)[:, :, 0:Tn]
                S.act(s5, pk, AF.Ln, bias=1e-24)
                S.act(s5, s5, AF.Exp, scale=-0.5)
                S.tt('dve', kk, kk, s5, ALU.mult)
                S.stt('dve', s5, aT, -1.0, bc(kac128, [128, 4, Tn], 2), ALU.add, ALU.mult)
                S.stt('dve', kp, s5, 1.0, kraw, ALU.add, ALU.mult)
                S.tt('pool', s5, rT, kp, ALU.mult)
                S.tt('pool', rkr[:, :, 0:Tn], s5, bc(rkc128, [128, 4, Tn], 2), ALU.mult)
                yield
                S.stt('dve', ARt[:, :, 0, 0:Tn], kk, -1.0, EA, ALU.mult, ALU.mult)
                S.tt('pool', ARt[:, :, 1, 0:Tn], rT, ER, ALU.mult)
                S.tt('dve', s5, kk, aT, ALU.mult)
                S.tt('dve', BtT[:, :, 0:Tn], s5, EB, ALU.mult)
                S.tt('pool', KtT[:, :, 0:Tn], kp, EB, ALU.mult)
                yield
                if slot == 15 or sample:
                    lo = 127 if slot == 15 else 0
                    M = 1 if slot == 15 else 64
                    for cb in range(7):
                        c0 = GLA_COLS + cb * 256
                        wb = wbuf[wi[0] % 3]; wi[0] += 1
                        S.dma(wb[:, :, :], winb_d[:, :, c0:c0 + 256])
                        p_ = pfA()
                        for k in range(8):
                            S.mm(p_[0:M, 0:256], nT[:, k, lo:lo + M], wb[:, k, :], start=(k == 0), stop=(k == 7))
                        S.cp('act', shrow[0:M, cb * 256:(cb + 1) * 256], p_[0:M, 0:256])
                    if slot == 15:
                        S.dma(shp_d, shrow[0:1, :], eng='act')
                    else:
                        for s_ in range(16):
                            S.dma(shs_d[s_:s_ + 1, :], shrow[4 * s_ + 3:4 * s_ + 4, :])
                yield

            HP = lambda h: slice(64 * (h % 2), 64 * (h % 2) + 64)

            def st2(slot, H):
                sample, Tn, tok0 = tile_info(slot)
                vT = H['vT']; ggT = H['ggT']; qs = H['qs']; ks = H['ks']; khT = H['khT']; gam = H['gam']; WCs = H['WCs']
                vTb = H['vTb']; ARt = H['ARt']; BtT = H['BtT']; KtT = H['KtT']; rkr = H['rkr']; sglb = H['sglb']
                chunks = [(s * 4, 4, s) for s in range(16)] if sample else [(0, Tn, None)]
                for ci, (o, Cn, seq) in enumerate(chunks):
                    tsl = slice(o, o + Cn)
                    if sample:
                        S.dma(Sg[:], sg_d[seq].rearrange("h k v -> k h v"))
                        S.cp('pool', Sgb[:], Sg[:])
                        srin = s2[3][0:64, :].rearrange("p (h j) -> p h j", h=8)
                        S.dma(srin, sr_d[seq].rearrange("h i j -> i h j"))
                        p_ = pfB()
                        for h in range(8):
                            S.tr(p_[0:64, h * 64:(h + 1) * 64], srin[:, h, :], identf[0:64, 0:64])
                        S.cp('dve', Sr[:], p_[0:64, :].rearrange("p (h i) -> p h i", h=8))
                        S.cp('act', Srb[:], p_[0:64, :].rearrange("p (h i) -> p h i", h=8))
                    p_ = psB[1]
                    for h in range(4):
                        S.tr(p_[0:Cn, h * 128:(h + 1) * 128], vT[:, h, tsl], identb[:, :])
                        S.tr(p_[0:Cn, 512 + h * 64:512 + (h + 1) * 64], khT[:, h, tsl], identb[0:64, 0:64])
                    S.cp('act', Vg[0:Cn, :], p_[0:Cn, 0:768])
                    p_ = pfB()
                    for h in range(4):
                        S.mm(p_[0:Cn, h * 128:h * 128 + Cn], ks[:, h, tsl], qs[:, h, tsl])
                    S.tt('dve', scT[0:Cn, :, 0:Cn], p_[0:Cn, :].rearrange("p (h t) -> p h t", h=4)[:, :, 0:Cn],
                         bc(triu_i[0:Cn, 0:Cn], [Cn, 4, Cn], 1), ALU.mult)
                    p_ = pfB()
                    for h in range(4):
                        S.mm(p_[:, h * 128:h * 128 + Cn], Vg[0:Cn, h * 128:(h + 1) * 128], scT[0:Cn, h, 0:Cn], start=True, stop=False)
                        S.mm(p_[:, h * 128:h * 128 + Cn], Sgb[:, h, :], qs[:, h, tsl], start=False, stop=True)
                    S.cp('act', ogT[:, :, tsl], p_[:, :].rearrange("p (h t) -> p h t", h=4)[:, :, 0:Cn])
                    p_ = pfB()
                    for h in range(4):
                        S.mm(p_[0:64, h * 128:(h + 1) * 128], Vg[0:Cn, 512 + h * 64:512 + (h + 1) * 64], Vg[0:Cn, h * 128:(h + 1) * 128])
                    sgt = v64v(s2[0])
                    S.tt('dve', sgt, Sg[:], bc(gam[:, :, ci], [64, 4, 128], 2), ALU.mult)
                    S.tt('dve', Sg[:], sgt, p_[0:64, :].rearrange("p (h v) -> p h v", h=4), ALU.add)
                    S.cp('act', Sgb[:], Sg[:])
                    if sample:
                        S.dma(glas_d[seq].rearrange("h k v -> k h v"), Sg[:])
                    yield
                    p_ = psB[1]
                    for c in range(4):
                        S.tr(p_[0:Cn, c * 128:(c + 1) * 128], ARt[:, c, 0, tsl], identb[:, :])
                        S.tr(p_[0:Cn, 512 + c * 128:512 + (c + 1) * 128], BtT[:, c, tsl], identb[:, :])
                    pv = p_[0:Cn, :].rearrange("p (a h j) -> p a h j", a=2, h=8)
                    S.cp('act', rhsPQ[0:Cn, :, 0, :], pv[:, 0, :, :])
                    S.cp('dve', tokB[0:Cn, :, :], pv[:, 1, :, :])
                    p_ = psB[1]
                    for c in range(4):
                        S.tr(p_[0:Cn, c * 128:(c + 1) * 128], KtT[:, c, tsl], identb[:, :])
                    for h in range(8):
                        S.tr(p_[0:Cn, 512 + h * 64:512 + (h + 1) * 64], vTb[:, h, tsl], identb[0:64, 0:64])
                    S.cp('act', tokKV[0:Cn, :, :, :], p_[0:Cn, :].rearrange("p (a h j) -> p a h j", a=2, h=8))
                    small = Cn <= 16
                    MhV = (lambda h: MhS[:, h]) if small else (lambda h: Mh[h])
                    if small:
                        for par in range(2):
                            p_ = pfB()
                            pm8 = p_[0:Cn, 0:256].rearrange("p (h q t) -> p h q t", h=4, q=4)
                            for hi in range(4):
                                h = 2 * hi + par
                                for a_ in range(2):
                                    S.mm(pm8[:, hi, a_, 0:Cn], BtT[HP(h), h // 2, tsl], ARt[HP(h), h // 2, a_, tsl])
                                    S.mm(pm8[:, hi, 2 + a_, 0:Cn], KtT[HP(h), h // 2, tsl], ARt[HP(h), h // 2, a_, tsl])
                            dstv = MhS[0:Cn, :, :, 0:Cn].rearrange("p (hi two) q t -> p hi two q t", two=2)[:, :, par]
                            S.tt('dve', dstv, pm8[:, :, :, 0:Cn], bc(m4[0:Cn, :, 0:Cn], [Cn, 4, 4, Cn], 1), ALU.mult)
                    else:
                        for h in range(8):
                            p_ = pfB()
                            pm = p_[0:Cn, :].rearrange("p (q t) -> p q t", q=4)
                            for a_ in range(2):
                                S.mm(pm[:, a_, 0:Cn], BtT[HP(h), h // 2, tsl], ARt[HP(h), h // 2, a_, tsl])
                                S.mm(pm[:, 2 + a_, 0:Cn], KtT[HP(h), h // 2, tsl], ARt[HP(h), h // 2, a_, tsl])
                            S.tt('dve', Mh[h][0:Cn, :, 0:Cn], pm[:, :, 0:Cn], m4[0:Cn, :, 0:Cn], ALU.mult)
                    yield
                    nlev = {4: 1, 16: 3, 128: 6}[Cn]
                    for half in range(2):
                        p_ = psB[1]
                        for hh in range(4):
                            h = half * 4 + hh
                            S.tr(p_[0:Cn, hh * 128:hh * 128 + Cn], MhV(h)[0:Cn, 0, 0:Cn], identb[0:Cn, 0:Cn])
                        S.cp('dve', YZ[half][0:Cn, :, 1, 0:Cn], p_[0:Cn, 0:512].rearrange("p (h s) -> p h s", h=4)[:, :, 0:Cn])
                        if small:
                            mh0 = MhS[0:Cn, 4 * half:4 * half + 4, 0, 0:Cn]
                            S.cp('pool', YZ[half][0:Cn, :, 0, 0:Cn], mh0)
                            S.tt('pool', TT[half][0:Cn, :, 0:Cn], mh0, bc(identb[0:Cn, 0:Cn], [Cn, 4, Cn], 1), ALU.add)
                        else:
                            for hh in range(4):
                                h = half * 4 + hh
                                S.cp('pool', YZ[half][0:Cn, hh, 0, 0:Cn], Mh[h][0:Cn, 0, 0:Cn])
                                S.tt('pool', TT[half][0:Cn, hh, 0:Cn], Mh[h][0:Cn, 0, 0:Cn], identb[0:Cn, 0:Cn], ALU.add)
                    yield
                    for lvl in range(nlev):
                        lastl = (lvl == nlev - 1)
                        for half in range(2):
                            yz = YZ[half]
                            if half == 0:
                                pyz = psW[0:Cn, :].rearrange("p (h a t) -> p h a t", h=4, a=2)
                                pY = pyz[:, :, 0, :]; pZ = pyz[:, :, 1, :]
                            else:
                                pY = psF[2][0:Cn, :].rearrange("p (h t) -> p h t", h=4)
                                pZ = psF[3][0:Cn, :].rearrange("p (h t) -> p h t", h=4)
                            for hh in range(4):
                                if not lastl:
                                    S.mm(pY[:, hh, 0:Cn], yz[0:Cn, hh, 1, 0:Cn], yz[0:Cn, hh, 0, 0:Cn])
                                S.mm(pZ[:, hh, 0:Cn], yz[0:Cn, hh, 0, 0:Cn], yz[0:Cn, hh, 1, 0:Cn])
                        for half in range(2):
                            yz = YZ[half]
                            if half == 0:
                                pyz = psW[0:Cn, :].rearrange("p (h a t) -> p h a t", h=4, a=2)
                                if lastl:
                                    S.cp('act', yz[0:Cn, :, 1, 0:Cn], pyz[:, :, 1, 0:Cn])
                                else:
                                    S.cp('act', yz[0:Cn, :, :, 0:Cn], pyz[:, :, :, 0:Cn])
                            else:
                                pY = psF[2][0:Cn, :].rearrange("p (h t) -> p h t", h=4)
                                pZ = psF[3][0:Cn, :].rearrange("p (h t) -> p h t", h=4)
                                S.cp('dve', yz[0:Cn, :, 1, 0:Cn], pZ[:, :, 0:Cn])
                                if not lastl:
                                    S.cp('act', yz[0:Cn, :, 0, 0:Cn], pY[:, :, 0:Cn])
                        for half in range(2):
                            yz = YZ[half]
                            p_ = psB1f
                            for hh in range(4):
                                S.mm(p_[0:Cn, hh * 128:hh * 128 + Cn], yz[0:Cn, hh, 1, 0:Cn], TT[half][0:Cn, hh, 0:Cn])
                            S.tt('dve', TT[half][0:Cn, :, 0:Cn], TT[half][0:Cn, :, 0:Cn],
                                 p_[0:Cn, :].rearrange("p (h t) -> p h t", h=4)[:, :, 0:Cn], ALU.add)
                        yield
                    p_ = pfB()
                    for h in range(8):
                        S.mm(p_[0:Cn, h * 64:(h + 1) * 64], MhV(h)[0:Cn, 2, 0:Cn], tokKV[0:Cn, 1, h, :])
                    S.cp('act', rhsPQ[0:Cn, :, 1, :], p_[0:Cn, :].rearrange("p (h i) -> p h i", h=8))
                    for h in range(8):
                        S.mm(psW[0:Cn, h * 128:(h + 1) * 128], TT[h // 4][0:Cn, h % 4, 0:Cn], rhsPQ[0:Cn, h, :, :].rearrange("p a j -> p (a j)"))
                    S.cp('act', PQ[0:Cn, :, :, :], psW[0:Cn, :].rearrange("p (h a j) -> p h a j", h=8, a=2))
                    yield
                    p_ = pfB()
                    for h in range(8):
                        S.mm(p_[0:64, h * 64:(h + 1) * 64], PQ[0:Cn, h, 0, :], tokB[0:Cn, h, :])
                    S.tt('dve', GT[:], p_[0:64, :].rearrange("p (h j) -> p h j", h=8), bc(identf[0:64, 0:64], [64, 8, 64], 1), ALU.add)
                    p_ = pfB()
                    for h in range(8):
                        S.mm(p_[0:64, h * 64:(h + 1) * 64], tokB[0:Cn, h, :], PQ[0:Cn, h, 1, :], start=True, stop=False)
                        S.mm(p_[0:64, h * 64:(h + 1) * 64], tokKV[0:Cn, 0, h, :], tokKV[0:Cn, 1, h, :], start=False, stop=True)
                    wcb = bc(WCs[:, :, ci], [64, 8, 64], 2)
                    S.tt('dve', HW[:], p_[0:64, :].rearrange("p (h i) -> p h i", h=8), wcb, ALU.mult)
                    for h in range(8):
                        S.mm(psW[0:64, h * 128:h * 128 + Cn], PQ[0:Cn, h, 0, :], MhV(h)[0:Cn, 1, 0:Cn], start=True, stop=False)
                        S.mm(psW[0:64, h * 128:h * 128 + Cn], identb[:, HP(h)], ARt[:, h // 2, 1, tsl], start=False, stop=True)
                    S.cp('act', ATb[:, :, 0:Cn], psW[0:64, :].rearrange("p (h t) -> p h t", h=8)[:, :, 0:Cn])
                    yield
                    for h in range(8):
                        o_ = psW[0:64, h * 128:h * 128 + Cn]
                        S.mm(o_, PQ[0:Cn, h, 1, :], MhV(h)[0:Cn, 1, 0:Cn], start=True, stop=False)
                        S.mm(o_, tokKV[0:Cn, 1, h, :], MhV(h)[0:Cn, 3, 0:Cn], start=False, stop=False)
                        S.mm(o_, Srb[:, h, :], ATb[:, h, 0:Cn], start=False, stop=True)
                    S.cp('act', yT[:, :, tsl], psW[0:64, :].rearrange("p (h t) -> p h t", h=8)[:, :, 0:Cn])
                    p_ = pfB()
                    for h in range(8):
                        S.mm(p_[0:64, h * 64:(h + 1) * 64], GT[:, h, :], Sr[:, h, :])
                    srt = s2[1][0:64, :].rearrange("p (h i) -> p h i", h=8)
                    S.tt('dve', srt, p_[0:64, :].rearrange("p (h i) -> p h i", h=8), wcb, ALU.mult)
                    S.tt('dve', Sr[:], srt, HW[:], ALU.add)
                    S.cp('act', Srb[:], Sr[:])
                    if sample or slot == 15:
                        p_ = pfB()
                        for h in range(8):
                            S.tr(p_[0:64, h * 64:(h + 1) * 64], Sr[:, h, :], identf[0:64, 0:64])
                        srout = s2[2][0:64, :].rearrange("p (h j) -> p h j", h=8)
                        S.cp('act', srout, p_[0:64, :].rearrange("p (h j) -> p h j", h=8))
                        dst = rws_d[seq] if sample else rwp_d
                        S.dma(dst.rearrange("h i j -> i h j"), srout, eng=('sp' if sample else 'act'))
                    yield
                if slot == 15:
                    S.dma(glap_d.rearrange("h k v -> k h v"), Sg[:], eng='act')
                if slot < 0:
                    return
                F = 4 * Tn
                og = ogT[:, :, 0:Tn]
                f3 = lambda t: t[:, 0:F].rearrange("p (h t) -> p h t", h=4)
                S.tt('pool', f3(s2[0]), og, og, ALU.mult)
                p_ = pfB()
                S.mm(p_[:, 0:F], onesf[:, :], s2[0][:, 0:F])
                S.act(s2[1][:, 0:F], p_[:, 0:F], AF.Ln, scale=1.0 / 128, bias=1e-6)
                S.act(s2[1][:, 0:F], s2[1][:, 0:F], AF.Exp, scale=-0.5)
                gg3 = ggT[:, :, 0:Tn]
                S.act(f3(s2[2]), gg3, AF.Exp, scale=-1.0)
                S.act(s2[2][:, 0:F], s2[2][:, 0:F], AF.Ln, bias=1.0)
                S.act(s2[2][:, 0:F], s2[2][:, 0:F], AF.Exp, scale=-1.0)
                S.tt('dve', f3(s2[2]), f3(s2[2]), gg3, ALU.mult)
                S.tt('dve', f3(s2[0]), og, f3(s2[1]), ALU.mult)
                S.stt('dve', actG[:, :, tok0:tok0 + Tn], f3(s2[0]), gnorm, f3(s2[2]), ALU.mult, ALU.mult)
                yield
                for half in range(2):
                    hs = slice(half * 4, half * 4 + 4)
                    ya = v64v(s2[3])[:, :, 0:Tn]; yb = v64v(s2[4])[:, :, 0:Tn]

                    def onesmm(lhs, srcf, rk=False):
                        p_ = pfB()
                        for hh in range(4):
                            if rk:
                                h = half * 4 + hh
                                S.mm(p_[0:64, hh * 128:hh * 128 + Tn], blkonesb[:, HP(h)], rkr[:, h // 2, 0:Tn])
                            else:
                                S.mm(p_[0:64, hh * 128:hh * 128 + Tn], lhs, srcf(hh))
                        return p_[0:64, :].rearrange("p (h t) -> p h t", h=4)[:, :, 0:Tn]
                    pm_ = onesmm(ones64[0:64, 0:64], lambda hh: yT[:, half * 4 + hh, 0:Tn])
                    S.tt('dve', ya, yT[:, hs, 0:Tn], pm_, ALU.subtract)
                    S.tt('pool', yb, ya, ya, ALU.mult)
                    pm_ = onesmm(ones64[0:64, 0:64], lambda hh: v64v(s2[4])[:, hh, 0:Tn])
                    S.act(yb, pm_, AF.Ln, bias=64e-5)
                    S.act(yb, yb, AF.Exp, scale=-0.5)
                    S.tt('dve', ya, ya, yb, ALU.mult)
                    S.tt('pool', ya, ya, bc(lnw[:, hs], [64, 4, Tn], 2), ALU.mult)
                    S.tt('pool', ya, ya, bc(lnb[:, hs], [64, 4, Tn], 2), ALU.add)
                    pm_ = onesmm(None, None, rk=True)
                    S.tt('dve', yb, pm_, vTb[:, hs, 0:Tn], ALU.mult)
                    S.tt('pool', ya, ya, yb, ALU.add)
                    p_ = pfB()
                    for hh in range(4):
                        h = half * 4 + hh
                        S.mm(p_[0:64, hh * 128:hh * 128 + Tn], g2b[:, h * 64:(h + 1) * 64], sglb[:, 0:Tn])
                    pg = p_[0:64, :].rearrange("p (c two t) -> p c two t", c=2, two=2)[:, :, :, 0:Tn]
                    ya4 = s2[3][0:64, :].rearrange("p (c two t) -> p c two t", c=2, two=2)[:, :, :, 0:Tn]
                    cs_ = slice(half * 2, half * 2 + 2)
                    S.tt('dve', actR[0:64, cs_, tok0:tok0 + Tn], ya4[:, :, 0, :], pg[:, :, 0, :], ALU.mult)
                    S.tt('dve', oRo[:, cs_, 0:Tn], ya4[:, :, 1, :], pg[:, :, 1, :], ALU.mult)
                    yield
                S.dma(actR[64:128, :, tok0:tok0 + Tn], oRo[:, :, 0:Tn], eng=('sp' if sample else 'act'))
                yield

            def run(g):
                for _ in g:
                    pass

            def interleave(ga, gb):
                live = [ga, gb]
                while live:
                    for g in list(live):
                        try:
                            next(g)
                        except StopIteration:
                            live.remove(g)
            slots = ([-1] + list(range(17)))[:K_NT]
            if slots:
                run(st1(slots[0], HB[0]))
            for i, slot in enumerate(slots):
                g2_ = st2(slot, HB[i % 2])
                if i + 1 < len(slots):
                    interleave(g2_, st1(slots[i + 1], HB[(i + 1) % 2]))
                else:
                    run(g2_)
            S.barrier()
            S.emit()

        xres = sbG("xres", [128, NSLOT, D])
        comb = sbG("comb", [128, NSLOT, NE])
        with ExitStack() as p2:
            sb = lambda n, s, d=F32: p2.enter_context(nc.sbuf_tensor("s_" + n, list(s), d))
            ps = lambda n, s, d=F32: p2.enter_context(nc.psum_tensor(n, list(s), d))
            psM = [ps("psM%d" % i, [128, 512]) for i in range(2)]
            psXs = [ps("psX%d" % i, [128, 1024]) for i in range(2)]
            psRs = [ps("psR%d" % i, [128, 512]) for i in range(2)]
            wo = sb("wo", [128, 8, D], BF16)
            stgo = [sb("stgo%d" % i, [128, 2, D]) for i in range(2)]
            for q in range(4):
                S.dma(stgo[q % 2][:], wout_d[:, 2 * q:2 * q + 2, :])
                S.cp(['pool', 'dve'][q % 2], wo[:, 2 * q:2 * q + 2, :], stgo[q % 2][:])
            wr = sb("wr", [128, 8, 36]); rb = sb("rb", [1, 36])
            S.dma(wr[:], wr_d); S.dma(rb[:], rb_d)
            B2 = []
            for i in range(2):
                B2.append(dict(junk2=sb("junk2_%d" % i, [128, D], BF16), xn=sb("xn%d" % i, [128, D]), xnT=sb("xnT%d" % i, [128, 8, 128]),
                               st2=sb("st2_%d" % i, [128, 4]), lg=sb("lg%d" % i, [128, 36]), r1=sb("r1_%d" % i, [128, 8]),
                               r2=sb("r2_%d" % i, [128, 8]), els=sb("els%d" % i, [128, 32]), mk1=sb("mk1_%d" % i, [128, 32]),
                               mk2=sb("mk2_%d" % i, [128, 32]), gm=sb("gm%d" % i, [128, 4])))
            def slot1b(slot):
                Sched.keymap = {'s_xres': ('xres', slot), 's_actG': ('actG', slot), 's_actR': ('actR', slot), 's_comb': ('comb', slot)}
                b_ = B2[slot % 2]
                junk2 = b_['junk2']; xn = b_['xn']; xnT = b_['xnT']; st2 = b_['st2']; lg = b_['lg']; r1 = b_['r1']
                r2 = b_['r2']; els = b_['els']; mk1 = b_['mk1']; mk2 = b_['mk2']; gm = b_['gm']
                psX = psXs[slot % 2]; psR = psRs[slot % 2]
                sample = (slot == 16)
                Tn = 64 if sample else 128
                tok0 = slot * 128
                src = xs_d if sample else x_d[slot * 128:(slot + 1) * 128, :]
                S.dma(xres[0:Tn, slot, :], src)
                for hf in range(2):
                    p_ = psM[hf]
                    for k in range(8):
                        a_ = actG if k < 4 else actR
                        S.mm(p_[0:Tn, :], a_[:, k % 4, tok0:tok0 + Tn], wo[:, k, hf * 512:(hf + 1) * 512], start=(k == 0), stop=(k == 7))
                    S.tt('dve', xres[0:Tn, slot, hf * 512:(hf + 1) * 512], xres[0:Tn, slot, hf * 512:(hf + 1) * 512], p_[0:Tn, :], ALU.add)
                S.memset('pool', st2[0:Tn, 0:1], 0.0)
                S.act(junk2[0:Tn, :], xres[0:Tn, slot, :], AF.Square, accum=st2[0:Tn, 0:1])
                S.act(st2[0:Tn, 1:2], st2[0:Tn, 0:1], AF.Ln, scale=1.0 / D, bias=1e-6)
                S.act(st2[0:Tn, 2:3], st2[0:Tn, 1:2], AF.Exp, scale=-0.5)
                S.act(xn[0:Tn, :], xres[0:Tn, slot, :], AF.Copy, scale=st2[0:Tn, 2:3])
                yield
                Sched.keymap = {'s_xres': ('xres', slot), 's_actG': ('actG', slot), 's_actR': ('actR', slot), 's_comb': ('comb', slot)}
                for k in range(8):
                    S.tr(psX[:, k * 128:k * 128 + Tn], xn[0:Tn, k * 128:(k + 1) * 128], identf[0:Tn, 0:Tn])
                S.tt('dve', xnT[:, :, 0:Tn], psX[:, :].rearrange("p (k t) -> p k t", k=8)[:, :, 0:Tn], bc(g_ffn, [128, 8, Tn], 2), ALU.mult)
                for k in range(8):
                    S.mm(psR[0:Tn, 0:36], xnT[:, k, 0:Tn], wr[:, k, :], start=(k == 0), stop=False)
                S.mm(psR[0:Tn, 0:36], onesf[0:1, 0:Tn], rb[0:1, :], start=False, stop=True)
                S.cp('act', lg[0:Tn, :], psR[0:Tn, 0:36])
                T_ = slice(0, Tn)
                S.op('dve', lambda e, T_=T_, r1=r1, lg=lg: e.tensor_reduce(r1[T_, 0:1], lg[T_, 0:4], AX.X, ALU.max), [lg], [r1])
                S.ts('dve', gm[T_, :], lg[T_, 0:4], r1[T_, 0:1], None, ALU.is_equal)
                S.ts('dve', r2[T_, 0:4], lg[T_, 0:4], r1[T_, 0:1], None, ALU.subtract)
                S.memset('pool', r1[T_, 1:2], 0.0)
                S.act(r2[T_, 0:4], r2[T_, 0:4], AF.Exp, accum=r1[T_, 1:2])
                S.recip(r1[T_, 2:3], r1[T_, 1:2])
                S.ts('dve', r2[T_, 4:8], gm[T_, :], -1.0, 1e30, ALU.add, ALU.mult)
                S.tt('dve', els[T_, :].rearrange("p (g e) -> p g e", g=4), lg[T_, 4:36].rearrange("p (g e) -> p g e", g=4),
                     bc(r2[T_, 4:8], [Tn, 4, 8], 2), ALU.add)
                S.op('dve', lambda e, T_=T_, r1=r1, els=els: e.tensor_reduce(r1[T_, 3:4], els[T_, :], AX.X, ALU.max), [els], [r1])
                S.ts('dve', mk1[T_, :], els[T_, :], r1[T_, 3:4], None, ALU.is_equal)
                S.stt('dve', els[T_, :], mk1[T_, :], -1e30, els[T_, :], ALU.mult, ALU.add)
                S.op('dve', lambda e, T_=T_, r1=r1, els=els: e.tensor_reduce(r1[T_, 4:5], els[T_, :], AX.X, ALU.max), [els], [r1])
                S.ts('dve', mk2[T_, :], els[T_, :], r1[T_, 4:5], None, ALU.is_equal)
                S.tt('dve', r1[T_, 5:6], r1[T_, 4:5], r1[T_, 3:4], ALU.subtract)
                S.act(r1[T_, 5:6], r1[T_, 5:6], AF.Exp)
                S.ts('dve', r1[T_, 5:6], r1[T_, 5:6], 1.0, None, ALU.add)
                S.recip(r1[T_, 6:7], r1[T_, 5:6])
                S.tt('dve', r1[T_, 6:7], r1[T_, 6:7], r1[T_, 2:3], ALU.mult)
                S.tt('dve', r1[T_, 7:8], r1[T_, 2:3], r1[T_, 6:7], ALU.subtract)
                S.ts('dve', mk1[T_, :], mk1[T_, :], r1[T_, 6:7], None, ALU.mult)
                S.stt('dve', comb[T_, slot, :], mk2[T_, :], r1[T_, 7:8], mk1[T_, :], ALU.mult, ALU.add)
                S.cp('pool', actG[:, :, tok0:tok0 + Tn], xnT[:, 0:4, 0:Tn])
                S.cp('act', actR[:, :, tok0:tok0 + Tn], xnT[:, 4:8, 0:Tn])

            n1b = NSLOT if K_PH >= 1 else 0
            g1b = [slot1b(s_) for s_ in range(n1b)]
            if n1b:
                next(g1b[0])
            for s_ in range(n1b):
                if s_ + 1 < n1b:
                    next(g1b[s_ + 1])
                for _ in g1b[s_]:
                    pass
            Sched.keymap = {}
            S.barrier()
            S.emit()

        with ExitStack() as p3:
            sb = lambda n, s, d=F32: p3.enter_context(nc.sbuf_tensor("s_" + n, list(s), d))
            ps = lambda n, s, d=F32: p3.enter_context(nc.psum_tensor(n, list(s), d))
            psA = [ps("psA%d" % i, [128, 512]) for i in range(2)]
            psBm = [ps("psBm%d" % i, [128, 512]) for i in range(2)]
            psY = [ps("psY%d" % i, [128, 512]) for i in range(4)]
            w1b = [sb("w1b%d" % i, [128, 8, DE], BF16) for i in range(2)]
            w3b = [sb("w3b%d" % i, [128, 8, DE], BF16) for i in range(2)]
            w2b_ = [sb("w2mb%d" % i, [128, 4, D], BF16) for i in range(2)]
            stg = [sb("mstg%d" % i, [128, 1024]) for i in range(4)]
            hT = [sb("hT%d" % i, [128, 4, 512], BF16) for i in range(2)]
            s1 = [sb("s1_%d" % i, [128, 512]) for i in range(2)]
            si = [0]

            def load_expert(e):
                b = e % 2
                for q in range(4):
                    st = stg[si[0] % 4]; si[0] += 1
                    S.dma(st[:, :].rearrange("p (k c) -> p k c", k=2), w1_d[e, :, 2 * q:2 * q + 2, :])
                    S.cp('pool', w1b[b][:, 2 * q:2 * q + 2, :], st[:, :].rearrange("p (k c) -> p k c", k=2))
                for q in range(4):
                    st = stg[si[0] % 4]; si[0] += 1
                    S.dma(st[:, :].rearrange("p (k c) -> p k c", k=2), w3_d[e, :, 2 * q:2 * q + 2, :])
                    S.cp('pool', w3b[b][:, 2 * q:2 * q + 2, :], st[:, :].rearrange("p (k c) -> p k c", k=2))
                for q in range(4):
                    st = stg[si[0] % 4]; si[0] += 1
                    S.dma(st[:, :], w2m_d[e, :, q, :])
                    S.cp('pool', w2b_[b][:, q, :], st[:, :])
            blocks = [(b * 512, 512, [4 * b + j for j in range(4)]) for b in range(4)] + [(2048, 64, [16])]
            cnt = [0]
            pend = [None]
            nexp = min(NE, MOE_LIMIT) if K_PH >= 2 else 0
            if nexp > 0:
                load_expert(0)
            for e in range(nexp):
                if pend[0] is not None:
                    pend[0](); pend[0] = None
                if e + 1 < nexp:
                    load_expert(e + 1)
                b = e % 2
                for (t0, nt, slots) in blocks:
                    hb = hT[cnt[0] % 2]
                    for hc in range(4):
                        if hc == 1 and pend[0] is not None:
                            pend[0](); pend[0] = None
                        pa = psA[hc % 2]; pb_ = psBm[hc % 2]
                        for k in range(8):
                            a_ = actG if k < 4 else actR
                            S.mm(pa[:, 0:nt], w1b[b][:, k, hc * 128:(hc + 1) * 128], a_[:, k % 4, t0:t0 + nt], start=(k == 0), stop=(k == 7))
                        for k in range(8):
                            a_ = actG if k < 4 else actR
                            S.mm(pb_[:, 0:nt], w3b[b][:, k, hc * 128:(hc + 1) * 128], a_[:, k % 4, t0:t0 + nt], start=(k == 0), stop=(k == 7))
                        s_ = s1[hc % 2]
                        S.act(s_[:, 0:nt], pa[:, 0:nt], AF.Silu)
                        S.tt('dve', hb[:, hc, 0:nt], s_[:, 0:nt], pb_[:, 0:nt], ALU.mult)
                    def ymm(hb=hb, slots=slots, nt=nt, b=b, e=e):
                        for j, slot in enumerate(slots):
                            Tn = min(128, nt)
                            for hf in range(2):
                                py = psY[(2 * j + hf) % 4]
                                for hc in range(4):
                                    S.mm(py[0:Tn, :], hb[:, hc, j * 128:j * 128 + Tn], w2b_[b][:, hc, hf * 512:(hf + 1) * 512], start=(hc == 0), stop=(hc == 3))
                                xr = xres[0:Tn, slot, hf * 512:(hf + 1) * 512]
                                S.stt('dve', xr, py[0:Tn, :], comb[0:Tn, slot, e:e + 1], xr, ALU.mult, ALU.add)
                    pend[0] = ymm
                    cnt[0] += 1
            if pend[0] is not None:
                pend[0](); pend[0] = None
            S.barrier()
            S.emit()

        with ExitStack() as p4:
            sb = lambda n, s, d=F32: p4.enter_context(nc.sbuf_tensor("s_" + n, list(s), d))
            nfb = sb("nfb", [128, D]); junk3s = [sb("junk3_%d" % i, [128, D], BF16) for i in range(2)]
            ob = [sb("ob%d" % i, [128, D]) for i in range(2)]
            st3s = [sb("st3_%d" % i, [128, 4]) for i in range(2)]
            S.dma(nfb[:], nf_d.partition_broadcast(128))
            for slot in range(NSLOT if K_PH >= 3 else 0):
                Tn = 64 if slot == 16 else 128
                Sched.keymap = {'s_xres': ('xres', slot)}
                st3 = st3s[slot % 2]; junk3 = junk3s[slot % 2]
                S.memset('pool', st3[0:Tn, 0:1], 0.0)
                S.act(junk3[0:Tn, :], xres[0:Tn, slot, :], AF.Square, accum=st3[0:Tn, 0:1])
                S.act(st3[0:Tn, 1:2], st3[0:Tn, 0:1], AF.Ln, scale=1.0 / D, bias=1e-6)
                S.act(st3[0:Tn, 2:3], st3[0:Tn, 1:2], AF.Exp, scale=-0.5)
                o_ = ob[slot % 2]
                S.stt('dve' if slot % 2 == 0 else 'pool', o_[0:Tn, :], xres[0:Tn, slot, :], st3[0:Tn, 2:3], nfb[0:Tn, :], ALU.mult, ALU.mult)
                dst = ys_d if slot == 16 else yp_d[slot * 128:(slot + 1) * 128, :]
                S.dma(dst, o_[0:Tn, :])
            Sched.keymap = {}
            S.finish()
            print("sem counts", {str(k): v for k, v in S.count.items()})
            S.emit()
    return nc


def _consts():
    c = np.zeros((128, 10, 128), np.float32)
    i = np.arange(128)
    s, t = np.meshgrid(i, i, indexing='ij')
    c[:, 0] = (s == t)
    c[:, 1] = (s <= t)
    c[:, 2] = (s < t)
    c[:, 3] = (s > t)
    c[:, 4] = 1.0
    same = (s // 4 == t // 4)
    c[:, 5] = same & (s <= t)
    c[:, 6] = same & (s < t)
    c[:, 7] = same & (s > t)
    c[:, 8] = 1.0 / 64
    c[:, 9] = (s // 64 == t // 64)
    return c


_NC_CACHE = {}


def kernel(x_prompt, x_sample, state_gla, state_rwkv, state_shift, meta_tokens, norm_mix, w_in,
           gla_gate_w2, gla_gate_b, gla_norm, rwkv_mu, rwkv_w0, rwkv_w2, rwkv_a0, rwkv_a2, rwkv_g2,
           rwkv_kk, rwkv_ka, rwkv_rk, rwkv_ln_w, rwkv_ln_b, w_out, norm_ffn, router_group_w,
           router_group_b, router_expert_w, router_expert_b, moe_w1, moe_w3, moe_w2, norm_final):
    f = lambda a: np.ascontiguousarray(np.asarray(a, dtype=np.float32))
    x_prompt = f(x_prompt); x_sample = f(x_sample)
    win = f(np.asarray(w_in)[0].reshape(8, 128, IN_COLS).transpose(1, 0, 2))
    wout = f(np.asarray(w_out)[0].reshape(8, 128, D).transpose(1, 0, 2))
    col128 = lambda v: np.asarray(v, np.float32).reshape(-1, 128).T
    col64 = lambda v: np.asarray(v, np.float32).reshape(-1, 64).T
    mu = np.asarray(rwkv_mu)[0]
    v128 = f(np.concatenate([col128(norm_mix[0]), col128(norm_ffn[0]), col128(gla_norm[0]), col128(mu[1664:1792]),
                             col128(mu[0:1024]), col128(mu[1536:1664]), col128(rwkv_a0[0]), col128(rwkv_kk[0]),
                             col128(rwkv_ka[0]), col128(np.asarray(rwkv_rk)[0].reshape(-1))], axis=1))
    v64 = f(np.concatenate([col64(mu[0:1664]), col64(rwkv_w0[0]), col64(rwkv_a0[0]), col64(rwkv_kk[0]), col64(rwkv_ka[0]),
                            col64(np.asarray(rwkv_rk)[0].reshape(-1)), col64(rwkv_ln_w[0]), col64(rwkv_ln_b[0])], axis=1))
    wr_full = np.concatenate([np.asarray(router_group_w)[0], np.asarray(router_expert_w)[0].transpose(1, 0, 2).reshape(D, 32)], axis=1)
    wr = f(wr_full.reshape(8, 128, 36).transpose(1, 0, 2))
    rb = f(np.concatenate([np.asarray(router_group_b)[0], np.asarray(router_expert_b)[0].reshape(-1)])[None, :])
    w1 = f(np.asarray(moe_w1)[0].reshape(NE, 8, 128, DE).transpose(0, 2, 1, 3))
    w3 = f(np.asarray(moe_w3)[0].reshape(NE, 8, 128, DE).transpose(0, 2, 1, 3))
    w2m = f(np.asarray(moe_w2)[0].reshape(NE, 4, 128, D).transpose(0, 2, 1, 3))
    shared = dict(meta=f(meta_tokens), win=win, wout=wout, gw2=f(gla_gate_w2[0]), gb=f(gla_gate_b), w2=f(rwkv_w2[0]),
                  a2=f(rwkv_a2[0]), g2=f(rwkv_g2[0]), w0r=f(rwkv_w0), v128=v128, v64=v64, wr=wr, rb=rb,
                  nf=f(np.asarray(norm_final)[None, :]), w1=w1, w3=w3, w2m=w2m, cst=_consts())
    in_maps = []
    for c in range(NCORES):
        m = dict(shared)
        m["x"] = x_prompt[c]
        m["xs"] = f(x_sample[16 * c:16 * c + 16].reshape(64, D))
        m["sg"] = f(state_gla[0, 16 * c:16 * c + 16])
        m["sr"] = f(state_rwkv[0, 16 * c:16 * c + 16])
        m["ssh"] = f(state_shift[0, 16 * c:16 * c + 16])
        in_maps.append(m)
    if "nc" not in _NC_CACHE:
        _NC_CACHE["nc"] = build()
    res = run_bass_kernel_spmd(_NC_CACHE["nc"], in_maps, core_ids=list(range(NCORES)))
    R = res.results
    y_prompt = np.stack([R[c]["yp"] for c in range(NCORES)], 0).astype(np.float32)
    y_sample = np.concatenate([R[c]["ys"].reshape(16, 4, D) for c in range(NCORES)], 0).astype(np.float32)
    gla_p = np.stack([R[c]["glap"] for c in range(NCORES)], 0)[None].astype(np.float32)
    rwkv_p = np.stack([R[c]["rwp"] for c in range(NCORES)], 0)[None].astype(np.float32)
    shift_p = np.concatenate([R[c]["shp"] for c in range(NCORES)], 0)[None].astype(np.float32)
    gla_s = np.concatenate([R[c]["glas"] for c in range(NCORES)], 0)[None].astype(np.float32)
    rwkv_s = np.concatenate([R[c]["rws"] for c in range(NCORES)], 0)[None].astype(np.float32)
    shift_s = np.concatenate([R[c]["shs"] for c in range(NCORES)], 0)[None].astype(np.float32)
    return (y_prompt, y_sample, gla_p, rwkv_p, shift_p, gla_s, rwkv_s, shift_s)
```
